# Optimizing a Trainium2 kernel written in Bass

```python
import jax, jax.numpy as jnp
from jax import lax
import numpy as np

D_MODEL = 1024
BATCH = 8
SEQ = 2048
DEPTH = 1

HEAD_DIM = 64
ROT_DIM = HEAD_DIM // 4
ROPE_THETA = 500000.0
EPS = 1e-6
NEG = -1e30

A_HEADS = 8
A_PATTERNS = ((128, 1), (512, 4), (2048, 16))
A_BLOCK = 128

B_HEADS = 8
B_KV_HEADS = 2
B_GROUP = B_HEADS // B_KV_HEADS
CMP_LEN = 32
CMP_STRIDE = 16
CMP_HIDDEN = 256
SEL_BLOCK = 64
SEL_TOPK = 8
WIN = 512
NSA_Q_BLOCK = 64
N_BRANCH = 3

A_WIDTH = A_HEADS * HEAD_DIM
B_WIDTH = B_HEADS * HEAD_DIM
KV_WIDTH = B_KV_HEADS * HEAD_DIM
MIX_WIDTH = A_WIDTH + B_WIDTH
IN_SIZES = (A_WIDTH, A_WIDTH, A_WIDTH, B_WIDTH, KV_WIDTH, KV_WIDTH, KV_WIDTH, KV_WIDTH, KV_WIDTH, KV_WIDTH, B_HEADS * N_BRANCH)
IN_WIDTH = 3 * A_WIDTH + B_WIDTH + 6 * KV_WIDTH + B_HEADS * N_BRANCH
D_FF = 4 * D_MODEL

kernel_name = 'hybrid_dilated_nsa_block'


def rms_norm(x, g):
    xf = x.astype(jnp.float32)
    y = xf * lax.rsqrt(jnp.mean(xf * xf, axis=-1, keepdims=True) + EPS)
    return (y * g.astype(jnp.float32)).astype(x.dtype)


def rope_cos_sin(pos):
    inv = ROPE_THETA ** (-jnp.arange(0, ROT_DIM, 2, dtype=jnp.float32) / ROT_DIM)
    ang = pos.astype(jnp.float32)[:, None] * inv[None, :]
    return jnp.cos(ang), jnp.sin(ang)


def apply_rope(x, cos, sin):
    half = ROT_DIM // 2
    xr = x[..., :ROT_DIM].astype(jnp.float32)
    x1, x2 = xr[..., :half], xr[..., half:]
    rot = jnp.concatenate([x1 * cos - x2 * sin, x2 * cos + x1 * sin], axis=-1)
    return jnp.concatenate([rot.astype(x.dtype), x[..., ROT_DIM:]], axis=-1)


def split_columns(proj):
    out, start = [], 0
    for n in IN_SIZES:
        out.append(proj[..., start:start + n])
        start += n
    return out


def to_heads(a, n):
    B, T, _ = a.shape
    return a.reshape(B, T, n, HEAD_DIM).transpose(0, 2, 1, 3)


def masked_softmax(s, mask):
    s = jnp.where(mask, s, NEG)
    m = jnp.max(s, axis=-1, keepdims=True)
    e = jnp.where(mask, jnp.exp(s - m), 0.0)
    den = jnp.sum(e, axis=-1, keepdims=True)
    return e / jnp.maximum(den, 1e-30)


def to_strided(a, dilation, sub_pad):
    B, H, T, hd = a.shape
    a = jnp.pad(a, ((0, 0), (0, 0), (0, sub_pad * dilation - T), (0, 0)))
    return a.reshape(B, H, sub_pad, dilation, hd).transpose(0, 1, 3, 2, 4)


def from_strided(a, T):
    B, H, d, S = a.shape[:4]
    a = jnp.moveaxis(a, 2, 3)
    return a.reshape((B, H, S * d) + a.shape[4:])[:, :, :T]


def dilated_window_attn(q, k, v, window, dilation):
    B, H, T, hd = q.shape
    n_back = window // dilation
    blk = A_BLOCK
    sub = -(-T // dilation)
    nb = -(-sub // blk)
    sub_pad = nb * blk
    qs = to_strided(q, dilation, sub_pad).reshape(B, H, dilation, nb, blk, hd)

    def band(a):
        ab = to_strided(a, dilation, sub_pad).reshape(B, H, dilation, nb, blk, hd)
        prev = jnp.pad(ab[:, :, :, :-1], ((0, 0), (0, 0), (0, 0), (1, 0), (0, 0), (0, 0)))
        return jnp.concatenate([prev, ab], axis=4)

    kb, vb = band(k), band(v)
    s = jnp.einsum('bhrnqd,bhrnkd->bhrnqk', qs, kb, preferred_element_type=jnp.float32) * (hd ** -0.5)
    qi = jnp.arange(blk)[:, None] + blk
    ki = jnp.arange(2 * blk)[None, :]
    dist = qi - ki
    mask = (dist >= 0) & (dist <= n_back) & ((ki >= blk) | (jnp.arange(nb)[:, None, None] > 0))
    s = jnp.where(mask, s, NEG)
    m = jnp.max(s, axis=-1)
    e = jnp.where(mask, jnp.exp(s - m[..., None]), 0.0)
    den = jnp.sum(e, axis=-1)
    o = jnp.einsum('bhrnqk,bhrnkd->bhrnqd', e, vb.astype(jnp.float32)) / den[..., None]
    o = from_strided(o.reshape(B, H, dilation, sub_pad, hd), T)
    m = from_strided(m.reshape(B, H, dilation, sub_pad), T)
    den = from_strided(den.reshape(B, H, dilation, sub_pad), T)
    return o, m, den


def dilated_mixture(q, k, v):
    res = [dilated_window_attn(q, k, v, w, d) for (w, d) in A_PATTERNS]
    m_all = res[0][1]
    for r in res[1:]:
        m_all = jnp.maximum(m_all, r[1])
    num, wsum = 0.0, 0.0
    for o, m, den in res:
        w = den * jnp.exp(m - m_all)
        num = num + w[..., None] * o
        wsum = wsum + w
    return (num / wsum[..., None]).astype(q.dtype)


def compress_tokens(a, pe, w1, w2):
    B, G, T, hd = a.shape
    nc = (T - CMP_LEN) // CMP_STRIDE + 1
    idx = jnp.arange(nc)[:, None] * CMP_STRIDE + jnp.arange(CMP_LEN)[None, :]
    blocks = a[:, :, idx] + pe
    hid = jax.nn.gelu(blocks.reshape(B, G, nc, CMP_LEN * hd) @ w1)
    return hid @ w2


def cmp_sel_overlap(nc, ns):
    c0 = jnp.arange(nc) * CMP_STRIDE
    s0 = jnp.arange(ns) * SEL_BLOCK
    ov = jnp.minimum(c0[:, None] + CMP_LEN, s0[None, :] + SEL_BLOCK) - jnp.maximum(c0[:, None], s0[None, :])
    return jnp.clip(ov, 0, None).astype(jnp.float32) / CMP_LEN


def nsa_attention(q, k_cmp, v_cmp, cmp_end, k_sel, v_sel, k_win, v_win, gates):
    B, G, R, T, hd = q.shape
    scale = hd ** -0.5
    ns = T // SEL_BLOCK
    n_sel = min(SEL_TOPK, ns)
    nqb = T // NSA_Q_BLOCK
    overlap = cmp_sel_overlap(k_cmp.shape[2], ns)
    k_sel_b = k_sel.reshape(B, G, ns, SEL_BLOCK, hd)
    v_sel_b = v_sel.reshape(B, G, ns, SEL_BLOCK, hd)
    pad = ((0, 0), (0, 0), (WIN, 0), (0, 0))
    k_win_p = jnp.pad(k_win, pad)
    v_win_p = jnp.pad(v_win, pad)
    gather_blocks = jax.vmap(jax.vmap(lambda blocks, ids: blocks[ids]))
    sblk = jnp.arange(ns)
    f32 = jnp.float32

    def one_block(b):
        t0 = b * NSA_Q_BLOCK
        qb = lax.dynamic_slice_in_dim(q, t0, NSA_Q_BLOCK, axis=3)
        gb = lax.dynamic_slice_in_dim(gates, t0, NSA_Q_BLOCK, axis=3)
        tpos = t0 + jnp.arange(NSA_Q_BLOCK)
        sc = jnp.einsum('bgrqd,bgcd->bgrqc', qb, k_cmp, preferred_element_type=f32) * scale
        p_cmp = masked_softmax(sc, cmp_end[None, :] <= tpos[:, None])
        o_cmp = jnp.einsum('bgrqc,bgcd->bgrqd', p_cmp, v_cmp.astype(f32))
        imp = jnp.einsum('bgrqc,cs->bgqs', p_cmp, overlap)
        cur = (tpos // SEL_BLOCK)[:, None]
        forced = (sblk == 0) | (sblk == cur) | (sblk == cur - 1)
        score = jnp.where(forced, imp + 2.0, jnp.where(sblk > cur, -1.0, imp))
        _, idx = lax.top_k(score, n_sel)
        kg = gather_blocks(k_sel_b, idx).reshape(B, G, NSA_Q_BLOCK, n_sel * SEL_BLOCK, hd)
        vg = gather_blocks(v_sel_b, idx).reshape(B, G, NSA_Q_BLOCK, n_sel * SEL_BLOCK, hd)
        kpos = (idx[..., None] * SEL_BLOCK + jnp.arange(SEL_BLOCK)).reshape(B, G, NSA_Q_BLOCK, n_sel * SEL_BLOCK)
        ss = jnp.einsum('bgrqd,bgqkd->bgrqk', qb, kg, preferred_element_type=f32) * scale
        p_sel = masked_softmax(ss, (kpos <= tpos[:, None])[:, :, None])
        o_sel = jnp.einsum('bgrqk,bgqkd->bgrqd', p_sel, vg.astype(f32))
        kw = lax.dynamic_slice_in_dim(k_win_p, t0, WIN + NSA_Q_BLOCK, axis=2)
        vw = lax.dynamic_slice_in_dim(v_win_p, t0, WIN + NSA_Q_BLOCK, axis=2)
        kpos_w = t0 - WIN + jnp.arange(WIN + NSA_Q_BLOCK)
        dist = tpos[:, None] - kpos_w[None, :]
        wmask = (dist >= 0) & (dist < WIN) & (kpos_w[None, :] >= 0)
        sw = jnp.einsum('bgrqd,bgkd->bgrqk', qb, kw, preferred_element_type=f32) * scale
        p_win = masked_softmax(sw, wmask)
        o_win = jnp.einsum('bgrqk,bgkd->bgrqd', p_win, vw.astype(f32))
        out = gb[..., 0:1] * o_cmp + gb[..., 1:2] * o_sel + gb[..., 2:3] * o_win
        return out.astype(q.dtype)

    outs = lax.map(one_block, jnp.arange(nqb))
    return outs.transpose(1, 0, 4, 2, 3, 5).reshape(B, T, G * R * hd)


def setup_inputs(seed: int = 0) -> dict:
    key = jax.random.key(seed)
    ks = jax.random.split(key, 16)
    f32 = jnp.float32
    L = DEPTH
    hd = HEAD_DIM

    def nrm(k, shape, fan_in):
        return jax.random.normal(k, shape, f32) * fan_in ** -0.5

    def gain(k, shape):
        return 1.0 + 0.02 * jax.random.normal(k, shape, f32)

    return {
        'x': jax.random.normal(ks[0], (BATCH, SEQ, D_MODEL), f32),
        'norm_mix': gain(ks[1], (L, D_MODEL)),
        'w_in': nrm(ks[2], (L, D_MODEL, IN_WIDTH), D_MODEL),
        'cmp_pe_k': 0.1 * jax.random.normal(ks[3], (L, CMP_LEN, hd), f32),
        'cmp_w1_k': nrm(ks[4], (L, CMP_LEN * hd, CMP_HIDDEN), CMP_LEN * hd),
        'cmp_w2_k': nrm(ks[5], (L, CMP_HIDDEN, hd), CMP_HIDDEN),
        'cmp_pe_v': 0.1 * jax.random.normal(ks[6], (L, CMP_LEN, hd), f32),
        'cmp_w1_v': nrm(ks[7], (L, CMP_LEN * hd, CMP_HIDDEN), CMP_LEN * hd),
        'cmp_w2_v': nrm(ks[8], (L, CMP_HIDDEN, hd), CMP_HIDDEN),
        'g_out_a': gain(ks[9], (L, A_WIDTH)),
        'g_out_b': gain(ks[10], (L, B_WIDTH)),
        'w_out': nrm(ks[11], (L, MIX_WIDTH, D_MODEL), MIX_WIDTH),
        'norm_mlp': gain(ks[12], (L, D_MODEL)),
        'w_up': nrm(ks[13], (L, D_MODEL, D_FF), D_MODEL),
        'w_down': nrm(ks[14], (L, D_FF, D_MODEL), D_FF),
        'norm_final': gain(ks[15], (D_MODEL,)),
    }


def reference(x, norm_mix, w_in, cmp_pe_k, cmp_w1_k, cmp_w2_k, cmp_pe_v, cmp_w1_v, cmp_w2_v, g_out_a, g_out_b, w_out, norm_mlp, w_up, w_down, norm_final):
    B, T, _ = x.shape
    cos, sin = rope_cos_sin(jnp.arange(T))
    nc = (T - CMP_LEN) // CMP_STRIDE + 1
    cmp_end = jnp.arange(nc) * CMP_STRIDE + CMP_LEN - 1
    cos_c, sin_c = rope_cos_sin(cmp_end)
    h_res = x
    for l in range(DEPTH):
        h = rms_norm(h_res, norm_mix[l])
        qa, ka, va, qb, kc, vc, ksl, vsl, kwn, vwn, gl = split_columns(h @ w_in[l])
        qa = apply_rope(to_heads(qa, A_HEADS), cos, sin)
        ka = apply_rope(to_heads(ka, A_HEADS), cos, sin)
        oa = dilated_mixture(qa, ka, to_heads(va, A_HEADS))
        oa = oa.transpose(0, 2, 1, 3).reshape(B, T, A_WIDTH)
        qn = apply_rope(to_heads(qb, B_HEADS), cos, sin).reshape(B, B_KV_HEADS, B_GROUP, T, HEAD_DIM)
        k_cmp = apply_rope(compress_tokens(to_heads(kc, B_KV_HEADS), cmp_pe_k[l], cmp_w1_k[l], cmp_w2_k[l]), cos_c, sin_c)
        v_cmp = compress_tokens(to_heads(vc, B_KV_HEADS), cmp_pe_v[l], cmp_w1_v[l], cmp_w2_v[l])
        k_sel = apply_rope(to_heads(ksl, B_KV_HEADS), cos, sin)
        v_sel = to_heads(vsl, B_KV_HEADS)
        k_win = apply_rope(to_heads(kwn, B_KV_HEADS), cos, sin)
        v_win = to_heads(vwn, B_KV_HEADS)
        gates = jax.nn.sigmoid(gl).reshape(B, T, B_KV_HEADS, B_GROUP, N_BRANCH).transpose(0, 2, 3, 1, 4)
        ob = nsa_attention(qn, k_cmp, v_cmp, cmp_end, k_sel, v_sel, k_win, v_win, gates)
        mixed = jnp.concatenate([rms_norm(oa, g_out_a[l]), rms_norm(ob, g_out_b[l])], axis=-1)
        h_res = h_res + mixed @ w_out[l]
        h = rms_norm(h_res, norm_mlp[l])
        h_res = h_res + jnp.square(jax.nn.relu(h @ w_up[l])) @ w_down[l]
    return rms_norm(h_res, norm_final)
```

```python
import numpy as np
from contextlib import ExitStack
import concourse.bass as bass
import concourse.mybir as mybir
from concourse.bass_utils import run_bass_kernel_spmd

F32 = mybir.dt.float32
BF16 = mybir.dt.bfloat16
ALU = mybir.AluOpType
AF = mybir.ActivationFunctionType

T = 2048
D = 1024
NT = 16
EPS = 1e-6
IN_W = 2840
ND = 6
NEGBIG = -30000.0


class Sched:
    def __init__(self, nc, ctx, same_engine_sync=True):
        self.nc = nc
        self.same = same_engine_sync
        self.eng = {'pe': nc.tensor, 'act': nc.scalar, 'dve': nc.vector, 'pool': nc.gpsimd, 'sp': nc.sync}
        self.thunks = {k: [] for k in self.eng}
        self.sem = {k: ctx.enter_context(nc.semaphore('prog_' + k)) for k in ['pe', 'act', 'dve', 'pool']}
        self.cnt = {k: 0 for k in self.sem}
        self.semobj = {}
        for k, s in self.sem.items():
            self.semobj[('c', k)] = s
        self.dq = ['sp', 'pool', 'act']
        self.dcnt = {}
        for q in self.dq:
            for i in range(ND):
                self.semobj[('d', q, i)] = ctx.enter_context(nc.semaphore('d_%s_%d' % (q, i)))
                self.dcnt[(q, i)] = 0
        self.dnext = {q: 0 for q in self.dq}
        self.seen = {k: {} for k in self.eng}
        self.res = {}
        self.nins = 0
        self.dead = False
        self.log = []

    def _deps(self, e, reads, writes):
        deps = {}

        def add(s, v):
            if v > deps.get(s, 0):
                deps[s] = v
        for r in reads:
            st = self.res.get(r)
            if st is not None and st[0] is not None:
                add(*st[0])
        for w in writes:
            st = self.res.get(w)
            if st is not None:
                if st[0] is not None:
                    add(*st[0])
                for s, v in st[1].items():
                    add(s, v)
        out = []
        for s, v in deps.items():
            if s == ('c', e) and (not self.same or e == 'pe'):
                continue
            if self.seen[e].get(s, 0) >= v:
                continue
            self.seen[e][s] = v
            out.append((s, v))
        return out

    def _mark(self, ev, reads, writes):
        for w in writes:
            self.res[w] = [ev, {}]
        s, v = ev
        for r in reads:
            st = self.res.get(r)
            if st is None:
                st = self.res[r] = [None, {}]
            if v > st[1].get(s, 0):
                st[1][s] = v

    def op(self, e, fn, reads=(), writes=()):
        if self.dead:
            return None
        waits = self._deps(e, reads, writes)
        self.cnt[e] += 1
        ev = (('c', e), self.cnt[e])
        sem = self.sem[e]
        semobj = self.semobj

        def thunk(engine):
            for s, v in waits:
                engine.wait_ge(semobj[s], v)
            fn(engine).then_inc(sem, 1)
        self.thunks[e].append(thunk)
        self.log.append((e, self.cnt[e], list(waits), list(reads), list(writes)))
        self._mark(ev, reads, writes)
        self.nins += 1
        return ev

    def dma(self, q, out, in_, reads=(), writes=(), **kw):
        if self.dead:
            return None
        i = self.dnext[q]
        self.dnext[q] = (i + 1) % ND
        waits = self._deps(q, reads, writes)
        s = ('d', q, i)
        prev = 16 * self.dcnt[(q, i)]
        if prev > 0 and self.seen[q].get(s, 0) < prev:
            self.seen[q][s] = prev
            waits.append((s, prev))
        self.dcnt[(q, i)] += 1
        ev = (s, 16 * self.dcnt[(q, i)])
        semobj = self.semobj

        def thunk(engine):
            for s2, v in waits:
                engine.wait_ge(semobj[s2], v)
            engine.dma_start(out=out, in_=in_, **kw).then_inc(semobj[s], 16)
        self.thunks[q].append(thunk)
        self._mark(ev, reads, writes)
        self.nins += 1
        return ev

    def all_events(self):
        evs = []
        for k in self.sem:
            if self.cnt[k] > 0:
                evs.append((('c', k), self.cnt[k]))
        for q in self.dq:
            for i in range(ND):
                if self.dcnt[(q, i)] > 0:
                    evs.append((('d', q, i), 16 * self.dcnt[(q, i)]))
        return evs

    def barrier(self, engines=('pe', 'act', 'dve', 'pool', 'sp')):
        if self.dead:
            return
        evs = self.all_events()
        semobj = self.semobj
        for e in engines:
            waits = []
            for s, v in evs:
                if self.seen[e].get(s, 0) >= v:
                    continue
                self.seen[e][s] = v
                waits.append((s, v))

            def thunk(engine, waits=waits):
                for s, v in waits:
                    engine.wait_ge(semobj[s], v)
            self.thunks[e].append(thunk)
        self.res = {}

    def emit(self):
        nc = self.nc
        evs = self.all_events()
        semobj = self.semobj

        def fin(engine):
            for s, v in evs:
                engine.wait_ge(semobj[s], v)
        self.thunks['sp'].append(fin)
        th = self.thunks
        with nc.Block() as block:
            @block.sync
            def _(eng):
                for t in th['sp']:
                    t(eng)

            @block.tensor
            def _(eng):
                for t in th['pe']:
                    t(eng)

            @block.scalar
            def _(eng):
                for t in th['act']:
                    t(eng)

            @block.vector
            def _(eng):
                for t in th['dve']:
                    t(eng)

            @block.gpsimd
            def _(eng):
                for t in th['pool']:
                    t(eng)


MA_W = 4352
MN_W = 3840
MA_OFF = {6: 0, 4: 1024, 2: 2048, 0: 3072, -2: 3968}
MN_WIN = [0, 1024, 1792, 2304]
MN_DIAG = [2560, 3456]


def _host_consts():
    c = {}
    inv = (500000.0 ** (-np.arange(0, 16, 2, dtype=np.float32) / 16)).astype(np.float32)
    pos = np.arange(T, dtype=np.float32)
    ang = pos[:, None] * inv[None, :]
    cos = np.cos(ang).astype(np.float32).reshape(NT, 128, 8).transpose(1, 0, 2)
    sin = np.sin(ang).astype(np.float32).reshape(NT, 128, 8).transpose(1, 0, 2)
    c['cosb'] = np.ascontiguousarray(np.broadcast_to(cos[:, :, None, :], (128, NT, 8, 8))).astype(np.float32)
    c['sinb'] = np.ascontiguousarray(np.broadcast_to(sin[:, :, None, :], (128, NT, 8, 8))).astype(np.float32)
    cend = (np.arange(128) * 16 + 31).astype(np.float32)
    angc = cend[:, None] * inv[None, :]
    c['cosc'] = np.ascontiguousarray(np.broadcast_to(np.cos(angc).astype(np.float32)[:, None, :], (128, 8, 8)))
    c['sinc'] = np.ascontiguousarray(np.broadcast_to(np.sin(angc).astype(np.float32)[:, None, :], (128, 8, 8)))
    ki = np.arange(128)[:, None]
    qi = np.arange(512)[None, :]

    def ca(delta):
        d = 128 * delta + qi - ki
        return ((d >= 0) & (d <= 128)).astype(np.float32) + ((d >= 0) & (d <= 512) & (d % 4 == 0)) + ((d >= 0) & (d % 16 == 0))

    def cz(delta):
        return ((128 * delta + qi - ki) >= 0).astype(np.float32)

    def wn(delta):
        d = 128 * delta + qi - ki
        return ((d >= 0) & (d < 512)).astype(np.float32)
    def addm(m):
        return np.where(m > 0, 8.0 * np.log(np.maximum(m, 1e-30)), NEGBIG).astype(np.float32)
    c['masksA'] = addm(np.concatenate([ca(8), ca(8), ca(4), ca(3), ca(2), ca(1), ca(0), ca(-1)[:, 128:], ca(-2)[:, 256:], ca(-3)[:, 384:]],
                                      axis=1))
    assert c['masksA'].shape == (128, MA_W)
    cm = []
    for qc in range(4):
        m = ((16 * ki + 31) <= (512 * qc + qi)).astype(np.float32) * np.ones((128, 512), np.float32)
        m[127, :] = 0
        cm.append(m)
    c['masksN'] = np.concatenate([addm(np.concatenate([wn(1), cz(0), wn(2)[:, 0:384], cz(-1)[:, 128:], wn(3)[:, 0:256], cz(-2)[:, 256:],
                                                       wn(4)[:, 0:128], cz(-3)[:, 384:]], axis=1)),
                                  cz(0), cz(-1)[:, 128:], cz(-2)[:, 256:], cz(-3)[:, 384:]], axis=1).astype(np.float32)
    c['maskcmp'] = np.concatenate(cm, axis=1).astype(np.float32)
    assert c['masksN'].shape == (128, MN_W)
    kk = np.arange(T)[None, :]
    ss = np.arange(32)[:, None]
    c['expand'] = ((kk // 64) == ss).astype(np.float32)
    c0 = np.arange(128) * 16
    s0 = np.arange(32) * 64
    ov = np.clip(np.minimum(c0[:, None] + 32, s0[None, :] + 64) - np.maximum(c0[:, None], s0[None, :]), 0, None) / 32.0
    ov[127, :] = 0
    c['ov'] = ov.astype(np.float32)
    tq = np.arange(T)
    cur = (tq // 64)[:, None]
    sb = np.arange(32)[None, :]
    forced = (sb == 0) | (sb == cur) | (sb == cur - 1)
    add = np.where(forced, 2.0, np.where(sb > cur, -1.0, 0.0)).astype(np.float32)
    c['addtab'] = np.ascontiguousarray(add.reshape(NT, 128, 32).transpose(1, 0, 2))
    return c


CONST_SHAPES = {
    'cosb': [128, NT, 8, 8], 'sinb': [128, NT, 8, 8], 'cosc': [128, 8, 8], 'sinc': [128, 8, 8],
    'masksA': [128, MA_W], 'masksN': [128, MN_W], 'maskcmp': [128, 2048], 'expand': [32, T], 'ov': [128, 32], 'addtab': [128, NT, 32],
}
W_SHAPES = {
    'norm_mix': [1, D], 'w_in': [D, IN_W], 'cmp_pe_k': [32, 64], 'cmp_w1_k': [2048, 256], 'cmp_w2_k': [256, 64],
    'cmp_pe_v': [32, 64], 'cmp_w1_v': [2048, 256], 'cmp_w2_v': [256, 64], 'g_out_a': [1, 512], 'g_out_b': [1, 512],
    'w_out': [D, D], 'norm_mlp': [1, D], 'w_up': [D, 4096], 'w_down': [4096, D], 'norm_final': [1, D],
}


def build_nc(stage=99, taps=None):
    nc = bass.Bass("TRN2", target_bir_lowering=False)
    dr = {}
    dr['x'] = nc.dram_tensor("x", [T, D], F32, kind="ExternalInput").ap()
    for k, shp in W_SHAPES.items():
        dr[k] = nc.dram_tensor(k, shp, F32, kind="ExternalInput").ap()
    for k, shp in CONST_SHAPES.items():
        dr[k] = nc.dram_tensor(k, shp, F32, kind="ExternalInput").ap()
    y = nc.dram_tensor("y", [T, D], F32, kind="ExternalOutput").ap()
    taps = taps or {}
    tap_dr = {k: nc.dram_tensor("tap_" + k, list(shp), F32, kind="ExternalOutput").ap() for k, shp in taps.items()}

    KB = 1024
    cnt = [0]

    def SB(name, shape, dt, off):
        cnt[0] += 1
        return nc.alloc_sbuf_tensor_at("%s_%d" % (name, cnt[0]), list(shape), dt, offset=int(off))

    class Arena:
        def __init__(self, base, limit):
            self.p = base
            self.limit = limit

        def a(self, name, shape, dt):
            nb = int(np.prod(shape[1:])) * (4 if dt == F32 else 2)
            nb = (nb + 63) // 64 * 64
            t = SB(name, shape, dt, self.p)
            self.p += nb
            assert self.p <= self.limit, (name, self.p, self.limit)
            return t

    with ExitStack() as ctx:
        S = Sched(nc, ctx)
        _CUT = 0

        def cut(n):
            if _CUT == n:
                S.dead = True
        pfall = ctx.enter_context(nc.psum_tensor("pfall", [128, 4096], F32))
        pf = [pfall[:, i * 512:(i + 1) * 512] for i in range(8)]
        pb = [pf[6][:, :].bitcast(BF16), pf[7][:, :].bitcast(BF16)]

        def MM(out, lhsT, rhs, start, stop, reads, writes):
            return S.op('pe', lambda e: e.matmul(out, lhsT=lhsT, rhs=rhs, start=start, stop=stop), reads, writes)

        def TR(out, in_, ident, reads, writes):
            return S.op('pe', lambda e: e.transpose(out=out, in_=in_, identity=ident), reads, writes)

        def ACT(out, in_, func, reads, writes, **kw):
            return S.op('act', lambda e: e.activation(out=out, in_=in_, func=func, **kw), reads, writes)

        def CP(eng, out, in_, reads, writes):
            if eng == 'act':
                return S.op('act', lambda e: e.copy(out=out, in_=in_), reads, writes)
            return S.op(eng, lambda e: e.tensor_copy(out=out, in_=in_), reads, writes)

        def TS(eng, out, in0, s1, s2, op0, op1, reads, writes):
            if op1 is None:
                return S.op(eng, lambda e: e.tensor_scalar(out=out, in0=in0, scalar1=s1, scalar2=None, op0=op0), reads, writes)
            return S.op(eng, lambda e: e.tensor_scalar(out=out, in0=in0, scalar1=s1, scalar2=s2, op0=op0, op1=op1), reads, writes)

        def TT(eng, out, in0, in1, op, reads, writes):
            return S.op(eng, lambda e: e.tensor_tensor(out=out, in0=in0, in1=in1, op=op), reads, writes)

        def STT(out, in0, scalar, in1, op0, op1, reads, writes):
            return S.op('dve', lambda e: e.scalar_tensor_tensor(out=out, in0=in0, scalar=scalar, in1=in1, op0=op0, op1=op1), reads, writes)

        BASE = 16512
        TOP = 229376
        Q = Arena(BASE, TOP)
        qaT = Q.a("qaT", [128, 4, T], BF16)
        kaT = Q.a("kaT", [128, 4, T], BF16)
        vA = Q.a("vA", [128, NT, 8, 65], BF16)
        qbT = Q.a("qbT", [128, 8, T], BF16)
        kselT = Q.a("kselT", [128, 2, T], BF16)
        kwinT = Q.a("kwinT", [128, 2, T], BF16)
        vsel = Q.a("vsel", [128, NT, 2, 65], BF16)
        vwin = Q.a("vwin", [128, NT, 2, 65], BF16)
        gates = Q.a("gates", [128, NT, 24], F32)
        QEND = Q.p
        C = Arena(QEND, TOP)
        identb = C.a("identb", [128, 128], BF16)
        identf = C.a("identf", [128, 128], F32)
        kcmpT = C.a("kcmpT", [128, 2, 128], BF16)
        VC = C.a("VC", [128, 2, 97], BF16)
        stat = C.a("stat", [128, 256], F32)
        epst = C.a("epst", [128, 16], F32)
        XB = C.p
        XL = TOP

        cut(1)
        X = Arena(XB, XL)
        kcT = X.a("kcT", [128, T], BF16)
        vcT = X.a("vcT", [128, T], BF16)
        xT = X.a("xT", [128, 8, T], BF16)
        Wg = [X.a("Wg%d" % i, [128, 8, 512], BF16) for i in range(2)]
        xin = [X.a("xin%d" % i, [128, D], F32) for i in range(3)]
        xnb = [X.a("xnb%d" % i, [128, D], BF16) for i in range(3)]
        gmix = X.a("gmix", [128, D], F32)
        cosb = X.a("cosb", [128, NT, 8, 8], F32)
        sinb = X.a("sinb", [128, NT, 8, 8], F32)
        stg = [X.a("stg%d" % i, [128, 512], BF16) for i in range(3)]
        stg2 = [X.a("stg2%d" % i, [128, 256], BF16) for i in range(3)]
        rtmp = [X.a("rtmp%d" % i, [128, 4, 8, 8], F32) for i in range(2)]
        ss = stat[:, 0:16]
        t1 = stat[:, 16:32]
        rstd = stat[:, 32:48]

        S.dma('sp', gmix[:], dr['norm_mix'].partition_broadcast(128), writes=['gmix'])
        S.dma('sp', cosb[:], dr['cosb'], writes=['cosb'])
        S.dma('sp', sinb[:], dr['sinb'], writes=['sinb'])
        w_in_v = dr['w_in'].rearrange("(kc p) n -> p kc n", p=128)
        groups = [(0, 512), (512, 512), (1024, 512), (1536, 512), (2048, 512), (2560, 280)]

        gorder = [2, 0, 1, 3, 4, 5]

        def load_wg(oi):
            c0, cw = groups[gorder[oi]]
            S.dma('pool', Wg[oi % 2][:, :, 0:cw], w_in_v[:, :, c0:c0 + cw], writes=[('Wg', oi % 2)])
        load_wg(0)
        load_wg(1)
        S.op('pool', lambda e: e.memset(epst[:], EPS), writes=['epst'])
        S.op('pool', lambda e: e.memset(identf[:], 0.0), writes=['identf'])
        S.op('pool', lambda e: e.affine_select(out=identf[:], in_=identf[:], pattern=[[-1, 128]], compare_op=ALU.not_equal,
                                              fill=1.0, base=0, channel_multiplier=1), reads=['identf'], writes=['identf'])
        CP('dve', identb[:], identf[:], ['identf'], ['identb'])
        S.op('pool', lambda e: e.memset(vA[:], 1.0), writes=['vA'])
        S.op('pool', lambda e: e.memset(vsel[:], 1.0), writes=['vsel'])
        S.op('pool', lambda e: e.memset(vwin[:], 1.0), writes=['vwin'])
        S.op('pool', lambda e: e.memset(kselT[64:128, :, :], 0.0), writes=[('ksexp', 0), ('ksexp', 1)])
        S.op('pool', lambda e: e.memset(kwinT[64:128, :, :], 0.0), writes=['kwz'])
        S.op('pool', lambda e: e.memset(kcmpT[64:128, :, :], 0.0), writes=['kcz'])
        S.op('pool', lambda e: e.memset(qbT[64:128, :, :], 0.0), writes=['qbneg'])
        for g in range(2):
            S.dma('pool', kselT[64:96, g, :], dr['expand'], writes=[('ksexp', g)])

        pbx = [pf[3][:, :].bitcast(BF16), pf[4][:, :].bitcast(BF16)]
        pby = [pb[0], pb[1], pf[5][:, :].bitcast(BF16)]
        pbyk = [('pb', 0), ('pb', 1), ('pb', 2)]

        def pre_a(tt):
            b = tt % 3
            S.dma('sp', xin[b][:], dr['x'][tt * 128:(tt + 1) * 128, :], writes=[('xin', b)])
            ACT(xnb[b][:], xin[b][:], AF.Square, [('xin', b)], [('xnb', b), ('ss', tt)], accum_out=ss[:, tt:tt + 1])
            ACT(t1[:, tt:tt + 1], ss[:, tt:tt + 1], AF.Sqrt, [('ss', tt), 'epst'], [('t1', tt)], scale=1.0 / D, bias=epst[:, 0:1])
            S.op('dve', lambda e, tt=tt: e.reciprocal(out=rstd[:, tt:tt + 1], in_=t1[:, tt:tt + 1]), [('t1', tt)], [('rstd', tt)])
            STT(xnb[b][:], xin[b][:], rstd[:, tt:tt + 1], gmix[:], ALU.mult, ALU.mult,
                [('xin', b), ('rstd', tt), 'gmix'], [('xnb', b)])

        def pre_b(tt):
            b = tt % 3
            pbb = tt % 2
            for kc in range(8):
                TR(pbx[pbb][:, kc * 128:(kc + 1) * 128], xnb[b][:, kc * 128:(kc + 1) * 128], identb[:],
                   [('xnb', b), 'identb'], [('pbx', pbb)])
            CP('act', xT[:, :, tt * 128:(tt + 1) * 128], pbx[pbb][:, :].rearrange("p (k t) -> p k t", k=8),
               [('pbx', pbb)], [('xT', tt)])

        cut(2)

        def rope_evac(src3, dst3, nh, tt, rb, rd, wr):
            cs = cosb[:, tt, 0:nh, :]
            sn = sinb[:, tt, 0:nh, :]
            x1 = src3[:, :, 0:8]
            x2 = src3[:, :, 8:16]
            ta = rtmp[rb][:, 0, 0:nh, :]
            tb = rtmp[rb][:, 1, 0:nh, :]
            tc = rtmp[rb][:, 2, 0:nh, :]
            td = rtmp[rb][:, 3, 0:nh, :]
            rk = ('rtmp', rb)
            TT('dve', ta, x1, cs, ALU.mult, rd + ['cosb'], [rk])
            TT('dve', tb, x2, sn, ALU.mult, rd + ['sinb'], [rk])
            TT('dve', tc, x2, cs, ALU.mult, rd, [rk])
            TT('dve', td, x1, sn, ALU.mult, rd, [rk])
            TT('dve', dst3[:, :, 0:8], ta, tb, ALU.subtract, [rk], wr)
            TT('dve', dst3[:, :, 8:16], tc, td, ALU.add, [rk], wr)
            CP('act', dst3[:, :, 16:64], src3[:, :, 16:64], rd, wr)

        pi = 0
        for tt0 in range(3):
            pre_a(tt0)
        pre_b(0)
        for oi, gi in enumerate(gorder):
            c0, cw = groups[gi]
            wgb = oi % 2
            posts = {}
            evacs = {}
            for tt in range(NT + 3):
                if tt < NT:
                    if oi == 0:
                        if tt + 3 < NT:
                            pre_a(tt + 3)
                        if tt + 1 < NT:
                            pre_b(tt + 1)
                    tsl = slice(tt * 128, (tt + 1) * 128)
                    bank = pf[pi % 3]
                    bk = ('pf', pi % 3)
                    pi += 1
                    for kc in range(8):
                        MM(bank[:, 0:cw], xT[:, kc, tsl], Wg[wgb][:, kc, 0:cw], kc == 0, kc == 7,
                           [('xT', tt), ('Wg', wgb)], [bk])
                    def evac(tt=tt, tsl=tsl, bank=bank, bk=bk):
                        sgb = tt % 3
                        pbk = pby[tt % 3]
                        pk = pbyk[tt % 3]
                        if gi in (0, 1, 3):
                            src3 = bank[:, 0:512].rearrange("p (h d) -> p h d", h=8)
                            dst3 = stg[sgb][:, :].rearrange("p (h d) -> p h d", h=8)
                            rope_evac(src3, dst3, 8, tt, tt % 2, [bk], [('stg', sgb)])
                            if gi in (0, 1):
                                def post(tt=tt, sgb=sgb, pbk=pbk, pk=pk, gi=gi, tsl=tsl):
                                    for hp in range(4):
                                        TR(pbk[:, hp * 128:(hp + 1) * 128], stg[sgb][:, hp * 128:(hp + 1) * 128], identb[:],
                                           [('stg', sgb), 'identb'], [pk])
                                    dstT = qaT if gi == 0 else kaT
                                    CP('act' if tt % 2 == 0 else 'dve', dstT[:, :, tsl], pbk[:, 0:512].rearrange("p (h t) -> p h t", h=4),
                                       [pk], [('qkT', gi, tt)])
                            else:
                                def post(tt=tt, sgb=sgb, pbk=pbk, pk=pk, tsl=tsl):
                                    for h in range(8):
                                        TR(pbk[0:64, h * 128:(h + 1) * 128], stg[sgb][:, h * 64:(h + 1) * 64], identb[:],
                                           [('stg', sgb), 'identb'], [pk])
                                    CP('act', qbT[0:64, :, tsl], pbk[0:64, :].rearrange("p (h t) -> p h t", h=8), [pk], [('qbT', tt)])
                            posts[tt] = post
                        elif gi == 2:
                            CP('act', vA[:, tt, :, 0:64], bank[:, 0:512].rearrange("p (h d) -> p h d", h=8), [bk, 'vA'], [('vA', tt)])
                        elif gi == 4:
                            rope_evac(bank[:, 0:512].rearrange("p (h d) -> p h d", h=8),
                                      stg[sgb][:, :].rearrange("p (h d) -> p h d", h=8), 8, tt, tt % 2, [bk], [('stg', sgb)])
                            CP('act', stg2[sgb][:, 0:256], bank[:, 0:256], [bk, ('stg', sgb)], [('stg2', sgb)])
                            CP('act', vsel[:, tt, :, 0:64], bank[:, 384:512].rearrange("p (h d) -> p h d", h=2), [bk, 'vsel', ('stg', sgb)], [('vsel', tt)])

                            def post(tt=tt, sgb=sgb, pbk=pbk, pk=pk, tsl=tsl):
                                rdl = [('stg2', sgb), ('stg', sgb), 'identb']
                                TR(pbk[:, 0:128], stg2[sgb][:, 0:128], identb[:], rdl, [pk])
                                TR(pbk[:, 128:256], stg2[sgb][:, 128:256], identb[:], rdl, [pk])
                                for g in range(2):
                                    TR(pbk[0:64, 256 + g * 128:384 + g * 128], stg[sgb][:, 256 + g * 64:320 + g * 64], identb[:], rdl, [pk])
                                CP('dve', kcT[:, tsl], pbk[:, 0:128], [pk], [('kcT', tt)])
                                CP('dve', vcT[:, tsl], pbk[:, 128:256], [pk], [('vcT', tt)])
                                CP('dve', kselT[0:64, :, tsl], pbk[0:64, 256:512].rearrange("p (g t) -> p g t", g=2), [pk], [('kselT', tt)])
                            posts[tt] = post
                        else:
                            rope_evac(bank[:, 0:512].rearrange("p (h d) -> p h d", h=8),
                                      stg[sgb][:, :].rearrange("p (h d) -> p h d", h=8), 8, tt, tt % 2, [bk], [('stg', sgb)])
                            CP('act', vwin[:, tt, :, 0:64], bank[:, 128:256].rearrange("p (h d) -> p h d", h=2), [bk, 'vwin', ('stg', sgb)], [('vwin', tt)])
                            ACT(gates[:, tt, :], bank[:, 256:280], AF.Sigmoid, [bk, ('stg', sgb)], [('gates', tt)])

                            def post(tt=tt, sgb=sgb, pbk=pbk, pk=pk, tsl=tsl):
                                rdl = [('stg', sgb), 'identb']
                                for g in range(2):
                                    TR(pbk[0:64, g * 128:(g + 1) * 128], stg[sgb][:, g * 64:(g + 1) * 64], identb[:], rdl, [pk])
                                CP('dve', kwinT[0:64, :, tsl], pbk[0:64, 0:256].rearrange("p (g t) -> p g t", g=2), [pk], [('kwinT', tt)])
                            posts[tt] = post
                    evacs[tt] = evac
                if tt - 1 in evacs:
                    evacs.pop(tt - 1)()
                if tt - 3 in posts:
                    posts.pop(tt - 3)()
            if oi + 2 < len(groups):
                load_wg(oi + 2)
            cut(10 + oi)

        S.barrier()
        dump = []
        if stage == 1:
            dump = [('qaT', qaT[:, :, :]), ('kaT', kaT[:, :, :]), ('vA', vA[:, :, :, :]), ('qbT', qbT[0:96, :, :]),
                    ('kselT', kselT[0:96, :, :]), ('kwinT', kwinT[:, :, :]), ('kcT', kcT[:, :]), ('gates', gates[:, :, :])]

        if stage >= 2:
            X = Arena(XB + 2 * T * 2, XL)
            W1 = [X.a("W1_%d" % i, [128, 32, 256], BF16) for i in range(2)]
            R = [X.a("R_%d" % i, [128, 32, 128], BF16) for i in range(2)]
            W2 = [X.a("W2_%d" % i, [128, 2, 64], BF16) for i in range(2)]
            pe2 = [X.a("pe2_%d" % i, [32, 128], F32) for i in range(2)]
            hidT = [[X.a("hidT_%d%d" % (i, g), [128, 2, 128], BF16) for g in range(2)] for i in range(2)]
            hb = X.a("hb", [128, 8], F32)
            kcs = X.a("kcs", [128, 8, 64], BF16)
            cosc = X.a("cosc", [128, 8, 8], F32)
            sinc = X.a("sinc", [128, 8, 8], F32)
            ovf = X.a("ovf", [128, 32], F32)
            rt2 = X.a("rt2", [128, 4, 8, 8], F32)
            P3OFF = XB + NT * D * 4
            assert X.p <= P3OFF, (X.p, P3OFF)
            masks = nc.alloc_sbuf_tensor_at("masksA", [128, MA_W], BF16, offset=P3OFF)
            addtab = nc.alloc_sbuf_tensor_at("addtab", [128, NT, 32], F32, offset=P3OFF + MA_W * 2)
            maskcmp = nc.alloc_sbuf_tensor_at("maskcmp", [128, 2048], BF16, offset=TOP - 4096 - 64)
            if stage >= 3:
                S.dma('pool', masks[:, :], dr['masksA'], writes=['masksA'])
                S.dma('pool', maskcmp[:, :], dr['maskcmp'], writes=['maskcmp'])
                S.dma('sp', addtab[:], dr['addtab'], writes=['addtab'])
            S.dma('sp', cosc[:], dr['cosc'], writes=['cosc'])
            S.dma('sp', sinc[:], dr['sinc'], writes=['sinc'])
            S.dma('sp', ovf[:], dr['ov'], writes=['ovf'])
            srcT = [kcT, vcT]
            for i, nm in enumerate(['k', 'v']):
                w1v = dr['cmp_w1_' + nm].rearrange("(j d) m -> d j m", d=64)
                for half in range(2):
                    S.dma('pool', W1[i][half * 64:(half + 1) * 64, :, :], w1v, writes=[('W1', i, half)])
                    S.dma('sp', pe2[i][0:32, half * 64:(half + 1) * 64], dr['cmp_pe_' + nm], writes=[('pe2', i, half)])
                S.dma('pool', W2[i][:, :, :], dr['cmp_w2_' + nm].rearrange("(mt p) d -> p mt d", p=128), writes=[('W2', i)])
                for a in range(2):
                    srcap = srcT[i][:, a * 16:a * 16 + 127 * 16].rearrange("p (c j) -> p j c", j=16)
                    CP('pool' if a == 0 else 'dve', R[i][:, a * 16:(a + 1) * 16, 0:127], srcap, [], [('R', i, a)])
                TR(pf[5][:, i * 32:(i + 1) * 32], pe2[i][0:32, :], identf[0:32, 0:32], [('pe2', i, 0), ('pe2', i, 1), 'identf'], ['pf5x'])
                CP('dve', R[i][:, :, 127], pf[5][:, i * 32:(i + 1) * 32], ['pf5x'], [('Rpe', i)])
            S.op('pool', lambda e: e.memset(VC[:, :, 64:65], 1.0), writes=['VC1'])
            for g in range(2):
                CP('dve', VC[:, g, 65:97], ovf[:, :], ['ovf'], [('VCov', g)])
            pcount = 0
            for i in range(2):
                for g in range(2):
                    gs = slice(g * 64, (g + 1) * 64)
                    for mt in range(2):
                        bank = pf[1 + pcount % 2]
                        bk = ('pf', 1 + pcount % 2)
                        pcount += 1
                        for j in range(32):
                            MM(bank[:, 0:128], W1[i][gs, j, mt * 128:(mt + 1) * 128], R[i][gs, j, :], j == 0, j == 31,
                               [('W1', i, g), ('R', i, 0), ('R', i, 1), ('Rpe', i)], [bk])
                        col = (i * 2 + g) * 2 + mt
                        CP('dve', hb[:, col:col + 1], bank[:, 127:128], [bk], [('hb', col)])
                        ACT(hidT[i][g][:, mt, :], bank[:, 0:128], AF.Gelu_apprx_tanh, [bk, ('hb', col)], [('hidT', i, g)],
                            bias=hb[:, col:col + 1])
                    if i == 0:
                        bank = pf[0]
                        bk = ('pf', 0)
                        for mt in range(2):
                            MM(bank[:, g * 64:(g + 1) * 64], hidT[i][g][:, mt, :], W2[i][:, mt, :], mt == 0, mt == 1,
                               [('hidT', i, g), ('W2', i)], [bk])
                    else:
                        bank = pf[3 + g]
                        bk = ('pf', 3 + g)
                        for mt in range(2):
                            MM(bank[:, 0:64], hidT[i][g][:, mt, :], W2[i][:, mt, :], mt == 0, mt == 1, [('hidT', i, g), ('W2', i)], [bk])
                        CP('act', VC[:, g, 0:64], bank[:, 0:64], [bk], [('VCv', g)])
                if i == 0:
                    bank = pf[0]
                    bk = ('pf', 0)
                    src3 = bank[:, 0:512].rearrange("p (h d) -> p h d", h=8)
                    dst3 = kcs[:, :, :]
                    rk = 'rt2'
                    cs = cosc[:, :, :]
                    sn = sinc[:, :, :]
                    x1 = src3[:, :, 0:8]
                    x2 = src3[:, :, 8:16]
                    ta, tb, tc, td = [rt2[:, q, :, :] for q in range(4)]
                    TT('dve', ta, x1, cs, ALU.mult, [bk, 'cosc'], [rk])
                    TT('dve', tb, x2, sn, ALU.mult, [bk, 'sinc'], [rk])
                    TT('dve', tc, x2, cs, ALU.mult, [bk], [rk])
                    TT('dve', td, x1, sn, ALU.mult, [bk], [rk])
                    TT('dve', dst3[:, :, 0:8], ta, tb, ALU.subtract, [rk], ['kcs'])
                    TT('dve', dst3[:, :, 8:16], tc, td, ALU.add, [rk], ['kcs'])
                    CP('act', dst3[:, :, 16:64], src3[:, :, 16:64], [bk], ['kcs'])
                    for g in range(2):
                        TR(pb[0][0:64, g * 128:(g + 1) * 128], kcs[:, g, :], identb[:], ['kcs', 'identb'], [('pb', 0)])
                    CP('act', kcmpT[0:64, :, :], pb[0][0:64, 0:256].rearrange("p (g c) -> p g c", g=2), [('pb', 0)], ['kcmpT'])
            S.barrier()
            if stage == 2:
                dump = [('kcmpT', kcmpT[:, :, :]), ('VC', VC[:, :, :])]

        if stage >= 3:
            X = Arena(XB, XL - 4096 - 64)
            big = X.a("big", [128, NT, D], F32)
            X.p += MA_W * 2 + NT * 32 * 4
            masksN = nc.alloc_sbuf_tensor_at("masksN", [128, MN_W], BF16, offset=BASE)
            NEP = 3
            Ep = [X.a("E%d" % i, [128, 1024], BF16) for i in range(NEP)]
            otsb = [X.a("otsb%d" % i, [128, 512], F32) for i in range(2)]
            imp = X.a("imp", [128, NT, 2, 32], F32)
            rec4 = [X.a("rec4_%d" % i, [128, 4], F32) for i in range(2)]
            w4 = [X.a("w4_%d" % i, [128, 4], F32) for i in range(2)]
            sc = [X.a("sc%d" % i, [128, 32], F32) for i in range(2)]
            m8 = [X.a("m8%d" % i, [128, 8], F32) for i in range(2)]
            negst = [X.a("negst%d" % i, [128, 96], BF16) for i in range(2)]
            qz = [[X.a("qz%d%d" % (a_, b_), [128, 512], BF16) for b_ in range(2)] for a_ in range(2)]
            ftmp1 = X.a("ftmp", [128, 4, 64], F32)
            ftmp = [ftmp1, ftmp1]
            itmp1 = X.a("itmp", [128, 4, 32], F32)
            itmp = [itmp1, itmp1]
            for a_ in range(2):
                for b_ in range(2):
                    S.op('pool', lambda e, a_=a_, b_=b_: e.memset(qz[a_][b_][:], 0.0), writes=[('qz', a_, b_)])
            for i in range(2):
                S.op('pool', lambda e, i=i: e.memset(negst[i][:], 0.0), writes=[('negst', i)])
            state = {'u': 0, 'ps': 0, 'pe': 0}
            pS4 = pfall[:, 0:2048]
            pO = [pf[4], pf[5]]
            pT = pf[6]
            pbs = pf[7][:, :].bitcast(BF16)

            def attn_branch(heads, kts_fn, lhsT_fn, rhs_fn, v_fn, M, mask_fn, fin_fn, extra_reads_fn=None, prep_fn=None,
                            unit_hook=None, v_key=None, cp_eng=None):
                units = [(h, qc) for h in heads for qc in range(4)]
                groups = []
                for ui_, (h, qc) in enumerate(units):
                    prs = kts_fn(qc)
                    u = state['u']
                    state['u'] += 1
                    for pi_, pr in enumerate(prs):
                        groups.append([h, qc, pr, pi_ == 0, pi_ == len(prs) - 1, u, ui_, None])
                if prep_fn is not None:
                    prep_fn(*units[0])
                LAGP = 2
                DEFER = 2
                pending = []
                n = len(groups)
                for i in range(n + LAGP):
                    if i < n:
                        h, qc, pr, first, last, u, ui_, _ = groups[i]
                        if first:
                            if prep_fn is not None and ui_ + 1 < len(units):
                                prep_fn(*units[ui_ + 1])
                            if unit_hook is not None:
                                unit_hook(ui_)
                        slot = state['ps'] % 2
                        state['ps'] += 1
                        eb = state['pe'] % NEP
                        state['pe'] += 1
                        groups[i][7] = eb
                        tl, mk = pr
                        w = sum(c1 - c0 for _, c0, c1 in tl)
                        xr = extra_reads_fn(h, qc) if extra_reads_fn else []
                        wa = tl[0][2] - tl[0][1]
                        split = len(tl) == 2 and wa < 512 and w > 512
                        wo_ = 0
                        addmode = mk is not None and len(mk) == 3
                        mo_ = 0
                        for kt, c0, c1 in tl:
                            dstp = pS4[:, slot * 1024 + wo_:slot * 1024 + wo_ + (c1 - c0)]
                            MM(dstp, lhsT_fn(h, kt), rhs_fn(h, qc)[:, c0:c1], True, not addmode, xr, [('pS', slot)])
                            if addmode:
                                MM(dstp, identb[:, :], mk[0][:, mo_:mo_ + (c1 - c0)], False, True, [mk[1], 'identb'], [('pS', slot)])
                            mo_ += c1 - c0
                            wo_ = 512 if split else wo_ + (c1 - c0)
                        if split:
                            assert w == 2 * wa
                            ACT(Ep[eb][:, 0:w].rearrange("p (t c) -> p t c", t=2),
                                pS4[:, slot * 1024:(slot + 1) * 1024].rearrange("p (t c) -> p t c", t=2)[:, :, 0:wa],
                                AF.Exp, [('pS', slot)], [('E', eb)], scale=0.125)
                        else:
                            ACT(Ep[eb][:, 0:w], pS4[:, slot * 1024:slot * 1024 + w], AF.Exp, [('pS', slot)], [('E', eb)], scale=0.125)
                        if mk is not None and not addmode:
                            mt_, mkey = mk
                            TT('dve', Ep[eb][:, 0:w], Ep[eb][:, 0:w], mt_, ALU.mult, [('E', eb), mkey], [('E', eb)])
                    j = i - LAGP
                    if j >= 0:
                        h, qc, pr, first, last, u, ui_, eb = groups[j]
                        ob = u % 2
                        tl, mk = pr
                        wo_ = 0
                        for t_, (kt, c0, c1) in enumerate(tl):
                            MM(pO[ob][0:M, c0:c1], v_fn(h, kt), Ep[eb][:, wo_:wo_ + (c1 - c0)],
                               first and t_ == 0, last and t_ == len(tl) - 1, [('E', eb)] + ([v_key] if v_key else []), [('pO', ob)])
                            wo_ += c1 - c0
                        if last:
                            for pi_ in [p_ for p_ in pending if p_[2] == ob]:
                                pending.remove(pi_)
                                pi_[1]()
                            CP(cp_eng or ('act' if u % 3 != 2 else 'dve'), otsb[ob][0:M, :], pO[ob][0:M, :], [('pO', ob)], [('otsb', ob)])

                            def fin_late(h=h, qc=qc, ob=ob):
                                for jj in range(4):
                                    TR(pT[:, jj * M:(jj + 1) * M], otsb[ob][0:M, jj * 128:(jj + 1) * 128], identf[0:M, 0:M],
                                       [('otsb', ob), 'identf'], ['pT'])
                                pT3 = pT[:, 0:4 * M].rearrange("p (j m) -> p j m", j=4)
                                TS('dve', rec4[ob][:, :], pT3[:, :, 64], 1e-30, None, ALU.max, None, ['pT'], [('rec4', ob)])
                                S.op('dve', lambda e, ob=ob: e.reciprocal(out=rec4[ob][:, :], in_=rec4[ob][:, :]),
                                     [('rec4', ob)], [('rec4', ob)])
                                fin_fn(h, qc, pT3, ob)
                            pending.append((i + DEFER, fin_late, ob))
                    while pending and pending[0][0] <= i:
                        pending.pop(0)[1]()
                while pending:
                    pending.pop(0)[1]()

            def bc4(ap4, n):
                return ap4.unsqueeze(2).to_broadcast([128, 4, n])

            def finA(h, qc, pT3, ob):
                TT('dve', big[:, 4 * qc:4 * qc + 4, h * 64:(h + 1) * 64], pT3[:, :, 0:64], bc4(rec4[ob][:, :], 64), ALU.mult,
                   ['pT', ('rec4', ob)], [('big', 4 * qc + jj, h) for jj in range(4)])

            def diag_pairs(qc, m0, m1):
                k0 = 4 * qc
                return [([(k0, 0, 512), (k0 + 1, 128, 512)], m0), ([(k0 + 2, 256, 512), (k0 + 3, 384, 512)], m1)]

            def pairsA(qc):
                prs = []
                for kt in range(0, 4 * qc, 2):
                    d0 = 4 * qc - kt
                    off = MA_OFF[6 if d0 >= 6 else d0]
                    prs.append(([(kt, 0, 512), (kt + 1, 0, 512)], (masks[:, off:off + 1024], 'masksA', 'add')))
                return prs + diag_pairs(qc, (masks[:, MA_OFF[0]:MA_OFF[0] + 896], 'masksA', 'add'), (masks[:, MA_OFF[-2]:MA_OFF[-2] + 384], 'masksA', 'add'))

            def pairs_sel(qc):
                prs = [([(kt, 0, 512), (kt + 1, 0, 512)], None) for kt in range(0, 4 * qc, 2)]
                return prs + diag_pairs(qc, (masksN[:, MN_DIAG[0]:MN_DIAG[0] + 896], 'masksN'), (masksN[:, MN_DIAG[1]:MN_DIAG[1] + 384], 'masksN'))

            def pairs_win(qc):
                if qc == 0:
                    return diag_pairs(qc, (masksN[:, MN_DIAG[0]:MN_DIAG[0] + 896], 'masksN'), (masksN[:, MN_DIAG[1]:MN_DIAG[1] + 384], 'masksN'))
                kd = lambda dl: 4 * qc - dl
                return [([(kd(1), 0, 512), (kd(0), 0, 512)], (masksN[:, MN_WIN[0]:MN_WIN[0] + 1024], 'masksN', 'add')),
                        ([(kd(2), 0, 384), (kd(-1), 128, 512)], (masksN[:, MN_WIN[1]:MN_WIN[1] + 768], 'masksN', 'add')),
                        ([(kd(3), 0, 256), (kd(-2), 256, 512)], (masksN[:, MN_WIN[2]:MN_WIN[2] + 512], 'masksN', 'add')),
                        ([(kd(4), 0, 128), (kd(-3), 384, 512)], (masksN[:, MN_WIN[3]:MN_WIN[3] + 256], 'masksN', 'add'))]

            def pairs_cmp(qc):
                return [([(0, 0, 512)], (maskcmp[:, 512 * qc:512 * (qc + 1)], 'maskcmp'))]

            def fin_nsa(br, is_first=False):
                def fin(h, qc, pT3, ob):
                    g = h // 4
                    r = h % 4
                    TT('dve', w4[ob][:, :], rec4[ob][:, :], gates[:, 4 * qc:4 * qc + 4, h * 3 + br], ALU.mult,
                       [('rec4', ob)], [('w4', ob)])
                    dst = big[:, 4 * qc:4 * qc + 4, 512 + h * 64:512 + (h + 1) * 64]
                    dk = [('big', 4 * qc + jj, 8 + h) for jj in range(4)]
                    if is_first:
                        TT('dve', dst, pT3[:, :, 0:64], bc4(w4[ob][:, :], 64), ALU.mult, ['pT', ('w4', ob)], dk)
                    else:
                        TT('dve', ftmp[ob][:, :, :], pT3[:, :, 0:64], bc4(w4[ob][:, :], 64), ALU.mult, ['pT', ('w4', ob)], ['ftmp'])
                        TT('pool', dst, dst, ftmp[ob][:, :, :], ALU.add, ['ftmp'] + dk, dk)
                    if br == 0:
                        idst = imp[:, 4 * qc:4 * qc + 4, g, :]
                        ik = [('imp', 4 * qc + jj, g) for jj in range(4)]
                        if r == 0:
                            TT('dve', idst, pT3[:, :, 65:97], bc4(rec4[ob][:, :], 32), ALU.mult, ['pT', ('rec4', ob)], ik)
                        else:
                            TT('dve', itmp[ob][:, :, :], pT3[:, :, 65:97], bc4(rec4[ob][:, :], 32), ALU.mult, ['pT', ('rec4', ob)], ['itmp'])
                            TT('pool', idst, idst, itmp[ob][:, :, :], ALU.add, ['itmp'] + ik, ik)
                return fin

            heads = list(range(8))
            attn_branch(heads, pairs_cmp,
                        lambda h, kt: kcmpT[:, h // 4, :],
                        lambda h, qc: qbT[:, h, qc * 512:(qc + 1) * 512],
                        lambda h, kt: VC[:, h // 4, :], 97, None, fin_nsa(0, True),
                        extra_reads_fn=lambda h, qc: [('neg', h // 4, 4 * qc + jj) for jj in range(4)], cp_eng='act')
            def selection_a(qt, g):
                sb_ = (qt * 2 + g) % 2
                TT('dve', sc[sb_][:, :], imp[:, qt, g, :], addtab[:, qt, :], ALU.add, [('imp', qt, g), 'addtab'], [('sc', sb_)])
                S.op('dve', lambda e, sb_=sb_: e.max(out=m8[sb_][:, :], in_=sc[sb_][:, :]), [('sc', sb_)], [('m8', sb_)])
                TS('dve', sc[sb_][:, :], sc[sb_][:, :], m8[sb_][:, 7:8], None, ALU.is_ge, None, [('sc', sb_), ('m8', sb_)], [('sc', sb_)])
                TS('dve', negst[sb_][:, 64:96], sc[sb_][:, :], -1.0, -NEGBIG, ALU.add, ALU.mult, [('sc', sb_)], [('negst', sb_)])

            def selection_b(qt, g):
                sb_ = (qt * 2 + g) % 2
                TR(pbs[0:96, 0:128], negst[sb_][:, 0:96], identb[:], [('negst', sb_), 'identb'], ['pbs'])
                for r in range(4):
                    CP('act', qbT[64:96, g * 4 + r, qt * 128:(qt + 1) * 128], pbs[64:96, 0:128],
                       ['pbs', 'qbneg'], [('neg', g, qt)])

            def win_hook(ui_):
                if ui_ >= 1:
                    selection_b((ui_ - 1) % NT, (ui_ - 1) // NT)
                selection_a(ui_ % NT, ui_ // NT)

            qzs = {'n': [0, 0], 'cur': {}}

            def prepA(h, qc):
                par = h % 2
                b_ = qzs['n'][par] % 2
                qzs['n'][par] += 1
                qzs['cur'][(h, qc)] = (par, b_)
                rows = slice(par * 64, par * 64 + 64)
                CP('pool', qz[par][b_][rows, :], qaT[rows, h // 2, qc * 512:(qc + 1) * 512], ['qaT'], [('qz', par, b_)])

            attn_branch(heads, pairsA,
                        lambda h, kt: kaT[:, h // 2, kt * 128:(kt + 1) * 128],
                        lambda h, qc: qz[qzs['cur'][(h, qc)][0]][qzs['cur'][(h, qc)][1]][:, :],
                        lambda h, kt: vA[:, kt, h, :], 65, None, finA,
                        extra_reads_fn=lambda h, qc: [('qz',) + qzs['cur'][(h, qc)]], prep_fn=prepA, v_key='vA_r', unit_hook=win_hook)
            Wo = nc.alloc_sbuf_tensor_at("Wo", [128, 8, D], BF16, offset=BASE + 2 * 4 * T * 2)
            if stage >= 4:
                wo_v = dr['w_out'].rearrange("(kc p) n -> p kc n", p=128)
                for kc in range(0, 8, 4):
                    S.dma('pool', Wo[:, kc:kc + 4, :], wo_v[:, kc:kc + 4, :], writes=['vA_r'])
            selection_b(NT - 1, 1)
            S.dma('pool', masksN[:, :], dr['masksN'], writes=['masksN', 'qaT'])
            attn_branch(heads, pairs_win,
                        lambda h, kt: kwinT[:, h // 4, kt * 128:(kt + 1) * 128],
                        lambda h, qc: qbT[:, h, qc * 512:(qc + 1) * 512],
                        lambda h, kt: vwin[:, kt, h // 4, :], 65, None, fin_nsa(2),
                        extra_reads_fn=lambda h, qc: [('neg', h // 4, 4 * qc + jj) for jj in range(4)], cp_eng='dve')
            attn_branch(heads, pairs_sel,
                        lambda h, kt: kselT[:, h // 4, kt * 128:(kt + 1) * 128],
                        lambda h, qc: qbT[:, h, qc * 512:(qc + 1) * 512],
                        lambda h, kt: vsel[:, kt, h // 4, :], 65, None, fin_nsa(1),
                        extra_reads_fn=lambda h, qc: [('neg', h // 4, 4 * qc + jj) for jj in range(4)], cp_eng='dve')
            S.barrier()
            if stage == 3:
                dump = [('big', big[:, :, :]), ('imp', imp[:, :, :, :]), ('qbT', qbT[0:96, :, :])]

        if stage >= 4:
            P = Arena(BASE, QEND)
            mixedT = P.a("mixedT", [128, 8, T], BF16)
            assert P.p == BASE + 2 * 4 * T * 2
            P.p += 8 * D * 2
            gab = P.a("gab", [128, D], F32)
            gmlp = P.a("gmlp", [128, D], F32)
            gfin = P.a("gfin", [128, D], F32)
            hnT = P.a("hnT", [128, 8, T], BF16)
            mixb = [P.a("mixb%d" % i, [128, D], BF16) for i in range(3)]
            mixb2 = [P.a("mixc%d" % i, [128, D], BF16) for i in range(3)]
            X2 = Arena(XB + NT * D * 4, XL)
            Wu = [X2.a("Wu0", [128, 8, 512], BF16), nc.alloc_sbuf_tensor_at("Wu1", [128, 8, 512], BF16, offset=BASE)]
            Wd = [X2.a("Wd0", [128, 4, D], BF16), nc.alloc_sbuf_tensor_at("Wd1", [128, 4, D], BF16, offset=BASE + 8192)]
            actT = [X2.a("actT%d" % i, [128, 4, 512], BF16) for i in range(2)]
            rl = [X2.a("rl%d" % i, [128, 512], BF16) for i in range(2)]
            junk2 = X2.a("junk2", [128, D], BF16)
            xres = [X2.a("xres0", [128, D], F32)]
            ssA = stat[:, 48:64]
            ssB = stat[:, 64:80]
            tA = stat[:, 80:112].rearrange("p (t k) -> p t k", k=2)
            rA = stat[:, 112:144].rearrange("p (t k) -> p t k", k=2)
            ss2 = stat[:, 144:160]
            t2 = stat[:, 160:176]
            r2 = stat[:, 176:192]
            ss3 = stat[:, 192:208]
            t3 = stat[:, 208:224]
            r3 = stat[:, 224:240]
            S.dma('sp', gab[:, 0:512], dr['g_out_a'].partition_broadcast(128), writes=['gab0'])
            S.dma('sp', gab[:, 512:1024], dr['g_out_b'].partition_broadcast(128), writes=['gab1'])
            S.dma('sp', gmlp[:], dr['norm_mlp'].partition_broadcast(128), writes=['gmlp'])
            S.dma('sp', gfin[:], dr['norm_final'].partition_broadcast(128), writes=['gfin'])
            wu_v = dr['w_up'].rearrange("(kc p) n -> p kc n", p=128)
            wd_v = dr['w_down'].rearrange("(fc p) n -> p fc n", p=128)
            allmt = [('mixedT', t_) for t_ in range(NT)]

            def load_mlp(fg):
                b = fg % 2
                extra = allmt if b == 1 else []
                S.dma('pool', Wu[b][:, :, :], wu_v[:, :, fg * 512:(fg + 1) * 512], writes=[('Wu', b)] + extra)
                S.dma('pool', Wd[b][:, :, :], wd_v[:, fg * 4:(fg + 1) * 4, :], writes=[('Wd', b)] + extra)
            load_mlp(0)

            def rstd_of(ssap, tap, rap, n, rd, wr):
                ACT(tap, ssap, AF.Sqrt, rd + ['epst'], wr, scale=1.0 / n, bias=epst[:, 0:1])
                S.op('dve', lambda e: e.reciprocal(out=rap, in_=tap), wr, wr)

            pbx = [pf[3][:, :].bitcast(BF16), pf[4][:, :].bitcast(BF16)]

            def st_A1(tt):
                bigr = [('big', tt, i) for i in range(16)]
                ACT(junk2[:, 0:512], big[:, tt, 0:512], AF.Square, bigr, ['junk2', ('ssA', tt)], accum_out=ssA[:, tt:tt + 1])
                ACT(junk2[:, 512:1024], big[:, tt, 512:1024], AF.Square, bigr, ['junk2', ('ssB', tt)], accum_out=ssB[:, tt:tt + 1])
                ACT(tA[:, tt, 0:1], ssA[:, tt:tt + 1], AF.Sqrt, [('ssA', tt), 'epst'], [('rA', tt, 0)], scale=1.0 / 512, bias=epst[:, 0:1])
                ACT(tA[:, tt, 1:2], ssB[:, tt:tt + 1], AF.Sqrt, [('ssB', tt), 'epst'], [('rA', tt, 1)], scale=1.0 / 512, bias=epst[:, 0:1])

            def st_A2(tt):
                b = tt % 3
                bigr = [('big', tt, i) for i in range(16)]
                S.op('dve', lambda e: e.reciprocal(out=rA[:, tt, :], in_=tA[:, tt, :]), [('rA', tt, 0), ('rA', tt, 1)], [('rA', tt, 0), ('rA', tt, 1)])
                STT(mixb[b][:, 0:512], big[:, tt, 0:512], rA[:, tt, 0:1], gab[:, 0:512], ALU.mult, ALU.mult,
                    bigr + [('rA', tt, 0), 'gab0'], [('mixb', b, 0)])
                STT(mixb[b][:, 512:1024], big[:, tt, 512:1024], rA[:, tt, 1:2], gab[:, 512:1024], ALU.mult, ALU.mult,
                    bigr + [('rA', tt, 1), 'gab1'], [('mixb', b, 1)])
                S.dma('sp', big[:, tt, :], dr['x'][tt * 128:(tt + 1) * 128, :], writes=bigr)

            def st_B1(tt):
                b = tt % 3
                pbb = tt % 2
                for kc in range(8):
                    TR(pbx[pbb][:, kc * 128:(kc + 1) * 128], mixb[b][:, kc * 128:(kc + 1) * 128], identb[:],
                       [('mixb', b, 0), ('mixb', b, 1), 'identb'], [('pf', 3 + pbb)])

            def st_B2(tt):
                pbb = tt % 2
                CP('act', mixedT[:, :, tt * 128:(tt + 1) * 128], pbx[pbb][:, :].rearrange("p (k t) -> p k t", k=8),
                   [('pf', 3 + pbb)], [('mixedT', tt)])

            opi = [0]

            obanks = [0, 1, 2, 5]
            cbank = {}

            def st_Cmm(tt):
                tsl = slice(tt * 128, (tt + 1) * 128)
                for cg in range(2):
                    bi = obanks[opi[0] % 4]
                    opi[0] += 1
                    cbank[(tt, cg)] = bi
                    for kc in range(8):
                        MM(pf[bi][:, :], mixedT[:, kc, tsl], Wo[:, kc, cg * 512:(cg + 1) * 512], kc == 0, kc == 7,
                           [('mixedT', tt), ('Wo', kc)], [('pf', bi)])

            def st_C1(tt):
                bigr = [('big', tt, i) for i in range(16)]
                for cg in range(2):
                    bi = cbank[(tt, cg)]
                    TT('dve', big[:, tt, cg * 512:(cg + 1) * 512], pf[bi][:, :], big[:, tt, cg * 512:(cg + 1) * 512], ALU.add,
                       [('pf', bi)] + bigr, [('hres', tt, cg)] + bigr)

            def st_C2(tt):
                hr = [('hres', tt, 0), ('hres', tt, 1)] + [('big', tt, i) for i in range(16)]
                ACT(junk2[:, :], big[:, tt, :], AF.Square, hr, ['junk2', ('ss2', tt)], accum_out=ss2[:, tt:tt + 1])
                ACT(t2[:, tt:tt + 1], ss2[:, tt:tt + 1], AF.Sqrt, [('ss2', tt), 'epst'], [('r2', tt)], scale=1.0 / D, bias=epst[:, 0:1])

            def st_C3(tt):
                b = tt % 3
                hr = [('hres', tt, 0), ('hres', tt, 1)] + [('big', tt, i) for i in range(16)]
                S.op('dve', lambda e: e.reciprocal(out=r2[:, tt:tt + 1], in_=t2[:, tt:tt + 1]), [('r2', tt)], [('r2', tt)])
                STT(mixb2[b][:, :], big[:, tt, :], r2[:, tt:tt + 1], gmlp[:, :], ALU.mult, ALU.mult,
                    hr + [('r2', tt), 'gmlp'], [('mixc', b)])

            def st_E1(tt):
                b = tt % 3
                pbb = tt % 2
                for kc in range(8):
                    TR(pb[pbb][:, kc * 128:(kc + 1) * 128], mixb2[b][:, kc * 128:(kc + 1) * 128], identb[:],
                       [('mixc', b), 'identb'], [('pb', pbb)])

            def st_E2(tt):
                pbb = tt % 2
                CP('act', hnT[:, :, tt * 128:(tt + 1) * 128], pb[pbb][:, :].rearrange("p (k t) -> p k t", k=8),
                   [('pb', pbb)], [('hnT', tt)])

            ui = 0
            di = 0

            def mlp_up(st):
                nonlocal ui
                fg, tc = st // 4, st % 4
                wb_ = fg % 2
                ab = st % 2
                hn_r = [('hnT', 4 * tc + jj) for jj in range(4)]
                for ft in range(4):
                    bank = pf[ui % 3]
                    bk = ('pf', ui % 3)
                    rb = ui % 2
                    ui += 1
                    for kc in range(8):
                        MM(bank[:, :], Wu[wb_][:, kc, ft * 128:(ft + 1) * 128], hnT[:, kc, tc * 512:(tc + 1) * 512],
                           kc == 0, kc == 7, hn_r + [('Wu', wb_)], [bk])
                    ACT(rl[rb][:, :], bank[:, :], AF.Relu, [bk], [('rl', rb)])
                    TT('pool', actT[ab][:, ft, :], rl[rb][:, :], rl[rb][:, :], ALU.mult, [('rl', rb)], [('actT', ab, ft)])

            def final_norm(tt):
                hr = [('hres', tt, 0), ('hres', tt, 1)]
                ACT(junk2[:, :], big[:, tt, :], AF.Square, hr, ['junk2', ('ss3', tt)], accum_out=ss3[:, tt:tt + 1])
                rstd_of(ss3[:, tt:tt + 1], t3[:, tt:tt + 1], r3[:, tt:tt + 1], D, [('ss3', tt)], [('r3', tt)])
                STT(big[:, tt, :], big[:, tt, :], r3[:, tt:tt + 1], gfin[:, :], ALU.mult, ALU.mult,
                    hr + [('r3', tt), 'gfin'], hr)
                S.dma('sp', y[tt * 128:(tt + 1) * 128, :], big[:, tt, :], reads=hr)

            def mlp_down(st):
                nonlocal di
                fg, tc = st // 4, st % 4
                wb_ = fg % 2
                ab = st % 2
                for jj in range(4):
                    tt = 4 * tc + jj
                    for cg in range(2):
                        bank = pf[3 + di % 3]
                        bk = ('pf', 3 + di % 3)
                        di += 1
                        for fc in range(4):
                            MM(bank[:, :], actT[ab][:, fc, jj * 128:(jj + 1) * 128], Wd[wb_][:, fc, cg * 512:(cg + 1) * 512],
                               fc == 0, fc == 3, [('actT', ab, fc), ('Wd', wb_)], [bk])
                        TT('dve', big[:, tt, cg * 512:(cg + 1) * 512], bank[:, :], big[:, tt, cg * 512:(cg + 1) * 512], ALU.add,
                           [bk, ('hres', tt, cg)], [('hres', tt, cg)])
                    if fg == 7:
                        final_norm(tt)

            def mlp_step(st):
                if st < 32:
                    mlp_up(st)
                if st >= 1:
                    mlp_down(st - 1)
                    if (st - 1) % 4 == 3 and (st - 1) // 4 + 2 < 8:
                        load_mlp((st - 1) // 4 + 2)

            nst = 0
            stages = [st_A1, st_A2, st_B1, st_B2, st_Cmm, st_C1, st_C2, st_C3, st_E1, st_E2]
            for it in range(NT + len(stages) - 1):
                for si, stg_fn in enumerate(stages):
                    tt = it - si
                    if 0 <= tt < NT:
                        stg_fn(tt)
            load_mlp(1)
            for st in range(nst, 33):
                mlp_step(st)

        for name, ap in dump:
            if name in tap_dr:
                S.dma('pool', tap_dr[name], ap)
        S.emit()
    nc._sched_log = S.log
    return nc


_CACHE = {}


def kernel(**inputs):
    consts = _host_consts()
    if 'nc' not in _CACHE:
        _CACHE['nc'] = build_nc()
    nc = _CACHE['nc']
    x = np.ascontiguousarray(np.asarray(inputs['x'], dtype=np.float32))
    shared = {}
    for k, shp in W_SHAPES.items():
        shared[k] = np.ascontiguousarray(np.asarray(inputs[k], dtype=np.float32).reshape(shp))
    for k in CONST_SHAPES:
        shared[k] = consts[k]
    in_maps = []
    for c in range(8):
        m = dict(shared)
        m['x'] = x[c]
        in_maps.append(m)
    res = run_bass_kernel_spmd(nc, in_maps, core_ids=list(range(8)))
    out = np.stack([np.asarray(res.results[c]['y'], dtype=np.float32) for c in range(8)], axis=0)
    return out
```

```python
import numpy as np
from contextlib import ExitStack
import concourse.bass as bass
import concourse.mybir as mybir
from concourse.bass_utils import run_bass_kernel_spmd

F32 = mybir.dt.float32
BF16 = mybir.dt.bfloat16
ALU = mybir.AluOpType
AF = mybir.ActivationFunctionType

T = 2048
D = 1024
NT = 16
EPS = 1e-6
IN_W = 2840
ND = 6
NEGBIG = -30000.0


class Sched:
    def __init__(self, nc, ctx, same_engine_sync=True):
        self.nc = nc
        self.same = same_engine_sync
        self.eng = {'pe': nc.tensor, 'act': nc.scalar, 'dve': nc.vector, 'pool': nc.gpsimd, 'sp': nc.sync}
        self.thunks = {k: [] for k in self.eng}
        self.sem = {k: ctx.enter_context(nc.semaphore('prog_' + k)) for k in ['pe', 'act', 'dve', 'pool']}
        self.cnt = {k: 0 for k in self.sem}
        self.semobj = {}
        for k, s in self.sem.items():
            self.semobj[('c', k)] = s
        self.dq = ['sp', 'pool', 'act']
        self.dcnt = {}
        for q in self.dq:
            for i in range(ND):
                self.semobj[('d', q, i)] = ctx.enter_context(nc.semaphore('d_%s_%d' % (q, i)))
                self.dcnt[(q, i)] = 0
        self.dnext = {q: 0 for q in self.dq}
        self.seen = {k: {} for k in self.eng}
        self.res = {}
        self.nins = 0
        self.dead = False
        self.log = []

    def _deps(self, e, reads, writes):
        deps = {}

        def add(s, v):
            if v > deps.get(s, 0):
                deps[s] = v
        for r in reads:
            st = self.res.get(r)
            if st is not None and st[0] is not None:
                add(*st[0])
        for w in writes:
            st = self.res.get(w)
            if st is not None:
                if st[0] is not None:
                    add(*st[0])
                for s, v in st[1].items():
                    add(s, v)
        out = []
        for s, v in deps.items():
            if s == ('c', e) and (not self.same or e == 'pe'):
                continue
            if self.seen[e].get(s, 0) >= v:
                continue
            self.seen[e][s] = v
            out.append((s, v))
        return out

    def _mark(self, ev, reads, writes):
        for w in writes:
            self.res[w] = [ev, {}]
        s, v = ev
        for r in reads:
            st = self.res.get(r)
            if st is None:
                st = self.res[r] = [None, {}]
            if v > st[1].get(s, 0):
                st[1][s] = v

    def op(self, e, fn, reads=(), writes=()):
        if self.dead:
            return None
        waits = self._deps(e, reads, writes)
        self.cnt[e] += 1
        ev = (('c', e), self.cnt[e])
        sem = self.sem[e]
        semobj = self.semobj

        def thunk(engine):
            for s, v in waits:
                engine.wait_ge(semobj[s], v)
            fn(engine).then_inc(sem, 1)
        self.thunks[e].append(thunk)
        self.log.append((e, self.cnt[e], list(waits), list(reads), list(writes)))
        self._mark(ev, reads, writes)
        self.nins += 1
        return ev

    def dma(self, q, out, in_, reads=(), writes=(), **kw):
        if self.dead:
            return None
        i = self.dnext[q]
        self.dnext[q] = (i + 1) % ND
        waits = self._deps(q, reads, writes)
        s = ('d', q, i)
        prev = 16 * self.dcnt[(q, i)]
        if prev > 0 and self.seen[q].get(s, 0) < prev:
            self.seen[q][s] = prev
            waits.append((s, prev))
        self.dcnt[(q, i)] += 1
        ev = (s, 16 * self.dcnt[(q, i)])
        semobj = self.semobj

        def thunk(engine):
            for s2, v in waits:
                engine.wait_ge(semobj[s2], v)
            engine.dma_start(out=out, in_=in_, **kw).then_inc(semobj[s], 16)
        self.thunks[q].append(thunk)
        self._mark(ev, reads, writes)
        self.nins += 1
        return ev

    def all_events(self):
        evs = []
        for k in self.sem:
            if self.cnt[k] > 0:
                evs.append((('c', k), self.cnt[k]))
        for q in self.dq:
            for i in range(ND):
                if self.dcnt[(q, i)] > 0:
                    evs.append((('d', q, i), 16 * self.dcnt[(q, i)]))
        return evs

    def barrier(self, engines=('pe', 'act', 'dve', 'pool', 'sp')):
        if self.dead:
            return
        evs = self.all_events()
        semobj = self.semobj
        for e in engines:
            waits = []
            for s, v in evs:
                if self.seen[e].get(s, 0) >= v:
                    continue
                self.seen[e][s] = v
                waits.append((s, v))

            def thunk(engine, waits=waits):
                for s, v in waits:
                    engine.wait_ge(semobj[s], v)
            self.thunks[e].append(thunk)
        self.res = {}

    def emit(self):
        nc = self.nc
        evs = self.all_events()
        semobj = self.semobj

        def fin(engine):
            for s, v in evs:
                engine.wait_ge(semobj[s], v)
        self.thunks['sp'].append(fin)
        th = self.thunks
        with nc.Block() as block:
            @block.sync
            def _(eng):
                for t in th['sp']:
                    t(eng)

            @block.tensor
            def _(eng):
                for t in th['pe']:
                    t(eng)

            @block.scalar
            def _(eng):
                for t in th['act']:
                    t(eng)

            @block.vector
            def _(eng):
                for t in th['dve']:
                    t(eng)

            @block.gpsimd
            def _(eng):
                for t in th['pool']:
                    t(eng)


MA_W = 4352
MN_W = 3840
MA_OFF = {6: 0, 4: 1024, 2: 2048, 0: 3072, -2: 3968}
MN_WIN = [0, 1024, 1792, 2304]
MN_DIAG = [2560, 3456]


def _host_consts():
    c = {}
    inv = (500000.0 ** (-np.arange(0, 16, 2, dtype=np.float32) / 16)).astype(np.float32)
    pos = np.arange(T, dtype=np.float32)
    ang = pos[:, None] * inv[None, :]
    cos = np.cos(ang).astype(np.float32).reshape(NT, 128, 8).transpose(1, 0, 2)
    sin = np.sin(ang).astype(np.float32).reshape(NT, 128, 8).transpose(1, 0, 2)
    c['cosb'] = np.ascontiguousarray(np.broadcast_to(cos[:, :, None, :], (128, NT, 8, 8))).astype(np.float32)
    c['sinb'] = np.ascontiguousarray(np.broadcast_to(sin[:, :, None, :], (128, NT, 8, 8))).astype(np.float32)
    cend = (np.arange(128) * 16 + 31).astype(np.float32)
    angc = cend[:, None] * inv[None, :]
    c['cosc'] = np.ascontiguousarray(np.broadcast_to(np.cos(angc).astype(np.float32)[:, None, :], (128, 8, 8)))
    c['sinc'] = np.ascontiguousarray(np.broadcast_to(np.sin(angc).astype(np.float32)[:, None, :], (128, 8, 8)))
    ki = np.arange(128)[:, None]
    qi = np.arange(512)[None, :]

    def ca(delta):
        d = 128 * delta + qi - ki
        return ((d >= 0) & (d <= 128)).astype(np.float32) + ((d >= 0) & (d <= 512) & (d % 4 == 0)) + ((d >= 0) & (d % 16 == 0))

    def cz(delta):
        return ((128 * delta + qi - ki) >= 0).astype(np.float32)

    def wn(delta):
        d = 128 * delta + qi - ki
        return ((d >= 0) & (d < 512)).astype(np.float32)
    def addm(m):
        return np.where(m > 0, 8.0 * np.log(np.maximum(m, 1e-30)), NEGBIG).astype(np.float32)
    c['masksA'] = addm(np.concatenate([ca(8), ca(8), ca(4), ca(3), ca(2), ca(1), ca(0), ca(-1)[:, 128:], ca(-2)[:, 256:], ca(-3)[:, 384:]],
                                      axis=1))
    assert c['masksA'].shape == (128, MA_W)
    cm = []
    for qc in range(4):
        m = ((16 * ki + 31) <= (512 * qc + qi)).astype(np.float32) * np.ones((128, 512), np.float32)
        m[127, :] = 0
        cm.append(m)
    c['masksN'] = np.concatenate([addm(np.concatenate([wn(1), cz(0), wn(2)[:, 0:384], cz(-1)[:, 128:], wn(3)[:, 0:256], cz(-2)[:, 256:],
                                                       wn(4)[:, 0:128], cz(-3)[:, 384:]], axis=1)),
                                  cz(0), cz(-1)[:, 128:], cz(-2)[:, 256:], cz(-3)[:, 384:]], axis=1).astype(np.float32)
    c['maskcmp'] = np.concatenate(cm, axis=1).astype(np.float32)
    assert c['masksN'].shape == (128, MN_W)
    kk = np.arange(T)[None, :]
    ss = np.arange(32)[:, None]
    c['expand'] = ((kk // 64) == ss).astype(np.float32)
    c0 = np.arange(128) * 16
    s0 = np.arange(32) * 64
    ov = np.clip(np.minimum(c0[:, None] + 32, s0[None, :] + 64) - np.maximum(c0[:, None], s0[None, :]), 0, None) / 32.0
    ov[127, :] = 0
    c['ov'] = ov.astype(np.float32)
    tq = np.arange(T)
    cur = (tq // 64)[:, None]
    sb = np.arange(32)[None, :]
    forced = (sb == 0) | (sb == cur) | (sb == cur - 1)
    add = np.where(forced, 2.0, np.where(sb > cur, -1.0, 0.0)).astype(np.float32)
    c['addtab'] = np.ascontiguousarray(add.reshape(NT, 128, 32).transpose(1, 0, 2))
    return c


CONST_SHAPES = {
    'cosb': [128, NT, 8, 8], 'sinb': [128, NT, 8, 8], 'cosc': [128, 8, 8], 'sinc': [128, 8, 8],
    'masksA': [128, MA_W], 'masksN': [128, MN_W], 'maskcmp': [128, 2048], 'expand': [32, T], 'ov': [128, 32], 'addtab': [128, NT, 32],
}
W_SHAPES = {
    'norm_mix': [1, D], 'w_in': [D, IN_W], 'cmp_pe_k': [32, 64], 'cmp_w1_k': [2048, 256], 'cmp_w2_k': [256, 64],
    'cmp_pe_v': [32, 64], 'cmp_w1_v': [2048, 256], 'cmp_w2_v': [256, 64], 'g_out_a': [1, 512], 'g_out_b': [1, 512],
    'w_out': [D, D], 'norm_mlp': [1, D], 'w_up': [D, 4096], 'w_down': [4096, D], 'norm_final': [1, D],
}


def build_nc(stage=99, taps=None):
    nc = bass.Bass("TRN2", target_bir_lowering=False)
    dr = {}
    dr['x'] = nc.dram_tensor("x", [T, D], F32, kind="ExternalInput").ap()
    for k, shp in W_SHAPES.items():
        dr[k] = nc.dram_tensor(k, shp, F32, kind="ExternalInput").ap()
    for k, shp in CONST_SHAPES.items():
        dr[k] = nc.dram_tensor(k, shp, F32, kind="ExternalInput").ap()
    y = nc.dram_tensor("y", [T, D], F32, kind="ExternalOutput").ap()
    taps = taps or {}
    tap_dr = {k: nc.dram_tensor("tap_" + k, list(shp), F32, kind="ExternalOutput").ap() for k, shp in taps.items()}

    KB = 1024
    cnt = [0]

    def SB(name, shape, dt, off):
        cnt[0] += 1
        return nc.alloc_sbuf_tensor_at("%s_%d" % (name, cnt[0]), list(shape), dt, offset=int(off))

    class Arena:
        def __init__(self, base, limit):
            self.p = base
            self.limit = limit

        def a(self, name, shape, dt):
            nb = int(np.prod(shape[1:])) * (4 if dt == F32 else 2)
            nb = (nb + 63) // 64 * 64
            t = SB(name, shape, dt, self.p)
            self.p += nb
            assert self.p <= self.limit, (name, self.p, self.limit)
            return t

    with ExitStack() as ctx:
        S = Sched(nc, ctx)
        _CUT = 0

        def cut(n):
            if _CUT == n:
                S.dead = True
        pfall = ctx.enter_context(nc.psum_tensor("pfall", [128, 4096], F32))
        pf = [pfall[:, i * 512:(i + 1) * 512] for i in range(8)]
        pb = [pf[6][:, :].bitcast(BF16), pf[7][:, :].bitcast(BF16)]

        def MM(out, lhsT, rhs, start, stop, reads, writes):
            return S.op('pe', lambda e: e.matmul(out, lhsT=lhsT, rhs=rhs, start=start, stop=stop), reads, writes)

        def TR(out, in_, ident, reads, writes):
            return S.op('pe', lambda e: e.transpose(out=out, in_=in_, identity=ident), reads, writes)

        def ACT(out, in_, func, reads, writes, **kw):
            return S.op('act', lambda e: e.activation(out=out, in_=in_, func=func, **kw), reads, writes)

        def CP(eng, out, in_, reads, writes):
            if eng == 'act':
                return S.op('act', lambda e: e.copy(out=out, in_=in_), reads, writes)
            return S.op(eng, lambda e: e.tensor_copy(out=out, in_=in_), reads, writes)

        def TS(eng, out, in0, s1, s2, op0, op1, reads, writes):
            if op1 is None:
                return S.op(eng, lambda e: e.tensor_scalar(out=out, in0=in0, scalar1=s1, scalar2=None, op0=op0), reads, writes)
            return S.op(eng, lambda e: e.tensor_scalar(out=out, in0=in0, scalar1=s1, scalar2=s2, op0=op0, op1=op1), reads, writes)

        def TT(eng, out, in0, in1, op, reads, writes):
            return S.op(eng, lambda e: e.tensor_tensor(out=out, in0=in0, in1=in1, op=op), reads, writes)

        def STT(out, in0, scalar, in1, op0, op1, reads, writes):
            return S.op('dve', lambda e: e.scalar_tensor_tensor(out=out, in0=in0, scalar=scalar, in1=in1, op0=op0, op1=op1), reads, writes)

        BASE = 16512
        TOP = 229376
        Q = Arena(BASE, TOP)
        qaT = Q.a("qaT", [128, 4, T], BF16)
        kaT = Q.a("kaT", [128, 4, T], BF16)
        vA = Q.a("vA", [128, NT, 8, 65], BF16)
        qbT = Q.a("qbT", [128, 8, T], BF16)
        kselT = Q.a("kselT", [128, 2, T], BF16)
        kwinT = Q.a("kwinT", [128, 2, T], BF16)
        vsel = Q.a("vsel", [128, NT, 2, 65], BF16)
        vwin = Q.a("vwin", [128, NT, 2, 65], BF16)
        gates = Q.a("gates", [128, NT, 24], F32)
        QEND = Q.p
        C = Arena(QEND, TOP)
        identb = C.a("identb", [128, 128], BF16)
        identf = C.a("identf", [128, 128], F32)
        kcmpT = C.a("kcmpT", [128, 2, 128], BF16)
        VC = C.a("VC", [128, 2, 97], BF16)
        stat = C.a("stat", [128, 256], F32)
        epst = C.a("epst", [128, 16], F32)
        XB = C.p
        XL = TOP

        cut(1)
        X = Arena(XB, XL)
        kcT = X.a("kcT", [128, T], BF16)
        vcT = X.a("vcT", [128, T], BF16)
        xT = X.a("xT", [128, 8, T], BF16)
        Wg = [X.a("Wg%d" % i, [128, 8, 512], BF16) for i in range(2)]
        xin = [X.a("xin%d" % i, [128, D], F32) for i in range(3)]
        xnb = [X.a("xnb%d" % i, [128, D], BF16) for i in range(3)]
        gmix = X.a("gmix", [128, D], F32)
        cosb = X.a("cosb", [128, NT, 8, 8], F32)
        sinb = X.a("sinb", [128, NT, 8, 8], F32)
        stg = [X.a("stg%d" % i, [128, 512], BF16) for i in range(3)]
        stg2 = [X.a("stg2%d" % i, [128, 256], BF16) for i in range(3)]
        rtmp = [X.a("rtmp%d" % i, [128, 4, 8, 8], F32) for i in range(2)]
        ss = stat[:, 0:16]
        t1 = stat[:, 16:32]
        rstd = stat[:, 32:48]

        S.dma('sp', gmix[:], dr['norm_mix'].partition_broadcast(128), writes=['gmix'])
        S.dma('sp', cosb[:], dr['cosb'], writes=['cosb'])
        S.dma('sp', sinb[:], dr['sinb'], writes=['sinb'])
        w_in_v = dr['w_in'].rearrange("(kc p) n -> p kc n", p=128)
        groups = [(0, 512), (512, 512), (1024, 512), (1536, 512), (2048, 512), (2560, 280)]

        gorder = [2, 0, 1, 3, 4, 5]

        def load_wg(oi):
            c0, cw = groups[gorder[oi]]
            S.dma('pool', Wg[oi % 2][:, :, 0:cw], w_in_v[:, :, c0:c0 + cw], writes=[('Wg', oi % 2)])
        load_wg(0)
        load_wg(1)
        S.op('pool', lambda e: e.memset(epst[:], EPS), writes=['epst'])
        S.op('pool', lambda e: e.memset(identf[:], 0.0), writes=['identf'])
        S.op('pool', lambda e: e.affine_select(out=identf[:], in_=identf[:], pattern=[[-1, 128]], compare_op=ALU.not_equal,
                                              fill=1.0, base=0, channel_multiplier=1), reads=['identf'], writes=['identf'])
        CP('dve', identb[:], identf[:], ['identf'], ['identb'])
        S.op('pool', lambda e: e.memset(vA[:], 1.0), writes=['vA'])
        S.op('pool', lambda e: e.memset(vsel[:], 1.0), writes=['vsel'])
        S.op('pool', lambda e: e.memset(vwin[:], 1.0), writes=['vwin'])
        S.op('pool', lambda e: e.memset(kselT[64:128, :, :], 0.0), writes=[('ksexp', 0), ('ksexp', 1)])
        S.op('pool', lambda e: e.memset(kwinT[64:128, :, :], 0.0), writes=['kwz'])
        S.op('pool', lambda e: e.memset(kcmpT[64:128, :, :], 0.0), writes=['kcz'])
        S.op('pool', lambda e: e.memset(qbT[64:128, :, :], 0.0), writes=['qbneg'])
        for g in range(2):
            S.dma('pool', kselT[64:96, g, :], dr['expand'], writes=[('ksexp', g)])

        pbx = [pf[3][:, :].bitcast(BF16), pf[4][:, :].bitcast(BF16)]
        pby = [pb[0], pb[1], pf[5][:, :].bitcast(BF16)]
        pbyk = [('pb', 0), ('pb', 1), ('pb', 2)]

        def pre_a(tt):
            b = tt % 3
            S.dma('sp', xin[b][:], dr['x'][tt * 128:(tt + 1) * 128, :], writes=[('xin', b)])
            ACT(xnb[b][:], xin[b][:], AF.Square, [('xin', b)], [('xnb', b), ('ss', tt)], accum_out=ss[:, tt:tt + 1])
            ACT(t1[:, tt:tt + 1], ss[:, tt:tt + 1], AF.Sqrt, [('ss', tt), 'epst'], [('t1', tt)], scale=1.0 / D, bias=epst[:, 0:1])
            S.op('dve', lambda e, tt=tt: e.reciprocal(out=rstd[:, tt:tt + 1], in_=t1[:, tt:tt + 1]), [('t1', tt)], [('rstd', tt)])
            STT(xnb[b][:], xin[b][:], rstd[:, tt:tt + 1], gmix[:], ALU.mult, ALU.mult,
                [('xin', b), ('rstd', tt), 'gmix'], [('xnb', b)])

        def pre_b(tt):
            b = tt % 3
            pbb = tt % 2
            for kc in range(8):
                TR(pbx[pbb][:, kc * 128:(kc + 1) * 128], xnb[b][:, kc * 128:(kc + 1) * 128], identb[:],
                   [('xnb', b), 'identb'], [('pbx', pbb)])
            CP('act', xT[:, :, tt * 128:(tt + 1) * 128], pbx[pbb][:, :].rearrange("p (k t) -> p k t", k=8),
               [('pbx', pbb)], [('xT', tt)])

        cut(2)

        def rope_evac(src3, dst3, nh, tt, rb, rd, wr):
            cs = cosb[:, tt, 0:nh, :]
            sn = sinb[:, tt, 0:nh, :]
            x1 = src3[:, :, 0:8]
            x2 = src3[:, :, 8:16]
            ta = rtmp[rb][:, 0, 0:nh, :]
            tb = rtmp[rb][:, 1, 0:nh, :]
            tc = rtmp[rb][:, 2, 0:nh, :]
            td = rtmp[rb][:, 3, 0:nh, :]
            rk = ('rtmp', rb)
            TT('dve', ta, x1, cs, ALU.mult, rd + ['cosb'], [rk])
            TT('dve', tb, x2, sn, ALU.mult, rd + ['sinb'], [rk])
            TT('dve', tc, x2, cs, ALU.mult, rd, [rk])
            TT('dve', td, x1, sn, ALU.mult, rd, [rk])
            TT('dve', dst3[:, :, 0:8], ta, tb, ALU.subtract, [rk], wr)
            TT('dve', dst3[:, :, 8:16], tc, td, ALU.add, [rk], wr)
            CP('act', dst3[:, :, 16:64], src3[:, :, 16:64], rd, wr)

        pi = 0
        for tt0 in range(3):
            pre_a(tt0)
        pre_b(0)
        for oi, gi in enumerate(gorder):
            c0, cw = groups[gi]
            wgb = oi % 2
            posts = {}
            evacs = {}
            for tt in range(NT + 3):
                if tt < NT:
                    if oi == 0:
                        if tt + 3 < NT:
                            pre_a(tt + 3)
                        if tt + 1 < NT:
                            pre_b(tt + 1)
                    tsl = slice(tt * 128, (tt + 1) * 128)
                    bank = pf[pi % 3]
                    bk = ('pf', pi % 3)
                    pi += 1
                    for kc in range(8):
                        MM(bank[:, 0:cw], xT[:, kc, tsl], Wg[wgb][:, kc, 0:cw], kc == 0, kc == 7,
                           [('xT', tt), ('Wg', wgb)], [bk])
                    def evac(tt=tt, tsl=tsl, bank=bank, bk=bk):
                        sgb = tt % 3
                        pbk = pby[tt % 3]
                        pk = pbyk[tt % 3]
                        if gi in (0, 1, 3):
                            src3 = bank[:, 0:512].rearrange("p (h d) -> p h d", h=8)
                            dst3 = stg[sgb][:, :].rearrange("p (h d) -> p h d", h=8)
                            rope_evac(src3, dst3, 8, tt, tt % 2, [bk], [('stg', sgb)])
                            if gi in (0, 1):
                                def post(tt=tt, sgb=sgb, pbk=pbk, pk=pk, gi=gi, tsl=tsl):
                                    for hp in range(4):
                                        TR(pbk[:, hp * 128:(hp + 1) * 128], stg[sgb][:, hp * 128:(hp + 1) * 128], identb[:],
                                           [('stg', sgb), 'identb'], [pk])
                                    dstT = qaT if gi == 0 else kaT
                                    CP('act' if tt % 2 == 0 else 'dve', dstT[:, :, tsl], pbk[:, 0:512].rearrange("p (h t) -> p h t", h=4),
                                       [pk], [('qkT', gi, tt)])
                            else:
                                def post(tt=tt, sgb=sgb, pbk=pbk, pk=pk, tsl=tsl):
                                    for h in range(8):
                                        TR(pbk[0:64, h * 128:(h + 1) * 128], stg[sgb][:, h * 64:(h + 1) * 64], identb[:],
                                           [('stg', sgb), 'identb'], [pk])
                                    CP('act', qbT[0:64, :, tsl], pbk[0:64, :].rearrange("p (h t) -> p h t", h=8), [pk], [('qbT', tt)])
                            posts[tt] = post
                        elif gi == 2:
                            CP('act', vA[:, tt, :, 0:64], bank[:, 0:512].rearrange("p (h d) -> p h d", h=8), [bk, 'vA'], [('vA', tt)])
                        elif gi == 4:
                            rope_evac(bank[:, 0:512].rearrange("p (h d) -> p h d", h=8),
                                      stg[sgb][:, :].rearrange("p (h d) -> p h d", h=8), 8, tt, tt % 2, [bk], [('stg', sgb)])
                            CP('act', stg2[sgb][:, 0:256], bank[:, 0:256], [bk, ('stg', sgb)], [('stg2', sgb)])
                            CP('act', vsel[:, tt, :, 0:64], bank[:, 384:512].rearrange("p (h d) -> p h d", h=2), [bk, 'vsel', ('stg', sgb)], [('vsel', tt)])

                            def post(tt=tt, sgb=sgb, pbk=pbk, pk=pk, tsl=tsl):
                                rdl = [('stg2', sgb), ('stg', sgb), 'identb']
                                TR(pbk[:, 0:128], stg2[sgb][:, 0:128], identb[:], rdl, [pk])
                                TR(pbk[:, 128:256], stg2[sgb][:, 128:256], identb[:], rdl, [pk])
                                for g in range(2):
                                    TR(pbk[0:64, 256 + g * 128:384 + g * 128], stg[sgb][:, 256 + g * 64:320 + g * 64], identb[:], rdl, [pk])
                                CP('dve', kcT[:, tsl], pbk[:, 0:128], [pk], [('kcT', tt)])
                                CP('dve', vcT[:, tsl], pbk[:, 128:256], [pk], [('vcT', tt)])
                                CP('dve', kselT[0:64, :, tsl], pbk[0:64, 256:512].rearrange("p (g t) -> p g t", g=2), [pk], [('kselT', tt)])
                            posts[tt] = post
                        else:
                            rope_evac(bank[:, 0:512].rearrange("p (h d) -> p h d", h=8),
                                      stg[sgb][:, :].rearrange("p (h d) -> p h d", h=8), 8, tt, tt % 2, [bk], [('stg', sgb)])
                            CP('act', vwin[:, tt, :, 0:64], bank[:, 128:256].rearrange("p (h d) -> p h d", h=2), [bk, 'vwin', ('stg', sgb)], [('vwin', tt)])
                            ACT(gates[:, tt, :], bank[:, 256:280], AF.Sigmoid, [bk, ('stg', sgb)], [('gates', tt)])

                            def post(tt=tt, sgb=sgb, pbk=pbk, pk=pk, tsl=tsl):
                                rdl = [('stg', sgb), 'identb']
                                for g in range(2):
                                    TR(pbk[0:64, g * 128:(g + 1) * 128], stg[sgb][:, g * 64:(g + 1) * 64], identb[:], rdl, [pk])
                                CP('dve', kwinT[0:64, :, tsl], pbk[0:64, 0:256].rearrange("p (g t) -> p g t", g=2), [pk], [('kwinT', tt)])
                            posts[tt] = post
                    evacs[tt] = evac
                if tt - 1 in evacs:
                    evacs.pop(tt - 1)()
                if tt - 3 in posts:
                    posts.pop(tt - 3)()
            if oi + 2 < len(groups):
                load_wg(oi + 2)
            cut(10 + oi)

        S.barrier()
        dump = []
        if stage == 1:
            dump = [('qaT', qaT[:, :, :]), ('kaT', kaT[:, :, :]), ('vA', vA[:, :, :, :]), ('qbT', qbT[0:96, :, :]),
                    ('kselT', kselT[0:96, :, :]), ('kwinT', kwinT[:, :, :]), ('kcT', kcT[:, :]), ('gates', gates[:, :, :])]

        if stage >= 2:
            X = Arena(XB + 2 * T * 2, XL)
            W1 = [X.a("W1_%d" % i, [128, 32, 256], BF16) for i in range(2)]
            R = [X.a("R_%d" % i, [128, 32, 128], BF16) for i in range(2)]
            W2 = [X.a("W2_%d" % i, [128, 2, 64], BF16) for i in range(2)]
            pe2 = [X.a("pe2_%d" % i, [32, 128], F32) for i in range(2)]
            hidT = [[X.a("hidT_%d%d" % (i, g), [128, 2, 128], BF16) for g in range(2)] for i in range(2)]
            hb = X.a("hb", [128, 8], F32)
            kcs = X.a("kcs", [128, 8, 64], BF16)
            cosc = X.a("cosc", [128, 8, 8], F32)
            sinc = X.a("sinc", [128, 8, 8], F32)
            ovf = X.a("ovf", [128, 32], F32)
            rt2 = X.a("rt2", [128, 4, 8, 8], F32)
            P3OFF = XB + NT * D * 4
            assert X.p <= P3OFF, (X.p, P3OFF)
            masks = nc.alloc_sbuf_tensor_at("masksA", [128, MA_W], BF16, offset=P3OFF)
            addtab = nc.alloc_sbuf_tensor_at("addtab", [128, NT, 32], F32, offset=P3OFF + MA_W * 2)
            maskcmp = nc.alloc_sbuf_tensor_at("maskcmp", [128, 2048], BF16, offset=TOP - 4096 - 64)
            if stage >= 3:
                S.dma('pool', masks[:, :], dr['masksA'], writes=['masksA'])
                S.dma('pool', maskcmp[:, :], dr['maskcmp'], writes=['maskcmp'])
                S.dma('sp', addtab[:], dr['addtab'], writes=['addtab'])
            S.dma('sp', cosc[:], dr['cosc'], writes=['cosc'])
            S.dma('sp', sinc[:], dr['sinc'], writes=['sinc'])
            S.dma('sp', ovf[:], dr['ov'], writes=['ovf'])
            srcT = [kcT, vcT]
            for i, nm in enumerate(['k', 'v']):
                w1v = dr['cmp_w1_' + nm].rearrange("(j d) m -> d j m", d=64)
                for half in range(2):
                    S.dma('pool', W1[i][half * 64:(half + 1) * 64, :, :], w1v, writes=[('W1', i, half)])
                    S.dma('sp', pe2[i][0:32, half * 64:(half + 1) * 64], dr['cmp_pe_' + nm], writes=[('pe2', i, half)])
                S.dma('pool', W2[i][:, :, :], dr['cmp_w2_' + nm].rearrange("(mt p) d -> p mt d", p=128), writes=[('W2', i)])
                for a in range(2):
                    srcap = srcT[i][:, a * 16:a * 16 + 127 * 16].rearrange("p (c j) -> p j c", j=16)
                    CP('pool' if a == 0 else 'dve', R[i][:, a * 16:(a + 1) * 16, 0:127], srcap, [], [('R', i, a)])
                TR(pf[5][:, i * 32:(i + 1) * 32], pe2[i][0:32, :], identf[0:32, 0:32], [('pe2', i, 0), ('pe2', i, 1), 'identf'], ['pf5x'])
                CP('dve', R[i][:, :, 127], pf[5][:, i * 32:(i + 1) * 32], ['pf5x'], [('Rpe', i)])
            S.op('pool', lambda e: e.memset(VC[:, :, 64:65], 1.0), writes=['VC1'])
            for g in range(2):
                CP('dve', VC[:, g, 65:97], ovf[:, :], ['ovf'], [('VCov', g)])
            pcount = 0
            for i in range(2):
                for g in range(2):
                    gs = slice(g * 64, (g + 1) * 64)
                    for mt in range(2):
                        bank = pf[1 + pcount % 2]
                        bk = ('pf', 1 + pcount % 2)
                        pcount += 1
                        for j in range(32):
                            MM(bank[:, 0:128], W1[i][gs, j, mt * 128:(mt + 1) * 128], R[i][gs, j, :], j == 0, j == 31,
                               [('W1', i, g), ('R', i, 0), ('R', i, 1), ('Rpe', i)], [bk])
                        col = (i * 2 + g) * 2 + mt
                        CP('dve', hb[:, col:col + 1], bank[:, 127:128], [bk], [('hb', col)])
                        ACT(hidT[i][g][:, mt, :], bank[:, 0:128], AF.Gelu_apprx_tanh, [bk, ('hb', col)], [('hidT', i, g)],
                            bias=hb[:, col:col + 1])
                    if i == 0:
                        bank = pf[0]
                        bk = ('pf', 0)
                        for mt in range(2):
                            MM(bank[:, g * 64:(g + 1) * 64], hidT[i][g][:, mt, :], W2[i][:, mt, :], mt == 0, mt == 1,
                               [('hidT', i, g), ('W2', i)], [bk])
                    else:
                        bank = pf[3 + g]
                        bk = ('pf', 3 + g)
                        for mt in range(2):
                            MM(bank[:, 0:64], hidT[i][g][:, mt, :], W2[i][:, mt, :], mt == 0, mt == 1, [('hidT', i, g), ('W2', i)], [bk])
                        CP('act', VC[:, g, 0:64], bank[:, 0:64], [bk], [('VCv', g)])
                if i == 0:
                    bank = pf[0]
                    bk = ('pf', 0)
                    src3 = bank[:, 0:512].rearrange("p (h d) -> p h d", h=8)
                    dst3 = kcs[:, :, :]
                    rk = 'rt2'
                    cs = cosc[:, :, :]
                    sn = sinc[:, :, :]
                    x1 = src3[:, :, 0:8]
                    x2 = src3[:, :, 8:16]
                    ta, tb, tc, td = [rt2[:, q, :, :] for q in range(4)]
                    TT('dve', ta, x1, cs, ALU.mult, [bk, 'cosc'], [rk])
                    TT('dve', tb, x2, sn, ALU.mult, [bk, 'sinc'], [rk])
                    TT('dve', tc, x2, cs, ALU.mult, [bk], [rk])
                    TT('dve', td, x1, sn, ALU.mult, [bk], [rk])
                    TT('dve', dst3[:, :, 0:8], ta, tb, ALU.subtract, [rk], ['kcs'])
                    TT('dve', dst3[:, :, 8:16], tc, td, ALU.add, [rk], ['kcs'])
                    CP('act', dst3[:, :, 16:64], src3[:, :, 16:64], [bk], ['kcs'])
                    for g in range(2):
                        TR(pb[0][0:64, g * 128:(g + 1) * 128], kcs[:, g, :], identb[:], ['kcs', 'identb'], [('pb', 0)])
                    CP('act', kcmpT[0:64, :, :], pb[0][0:64, 0:256].rearrange("p (g c) -> p g c", g=2), [('pb', 0)], ['kcmpT'])
            S.barrier()
            if stage == 2:
                dump = [('kcmpT', kcmpT[:, :, :]), ('VC', VC[:, :, :])]

        if stage >= 3:
            X = Arena(XB, XL - 4096 - 64)
            big = X.a("big", [128, NT, D], F32)
            X.p += MA_W * 2 + NT * 32 * 4
            masksN = nc.alloc_sbuf_tensor_at("masksN", [128, MN_W], BF16, offset=BASE)
            NEP = 3
            Ep = [X.a("E%d" % i, [128, 1024], BF16) for i in range(NEP)]
            otsb = [X.a("otsb%d" % i, [128, 512], F32) for i in range(2)]
            imp = X.a("imp", [128, NT, 2, 32], F32)
            rec4 = [X.a("rec4_%d" % i, [128, 4], F32) for i in range(2)]
            w4 = [X.a("w4_%d" % i, [128, 4], F32) for i in range(2)]
            sc = [X.a("sc%d" % i, [128, 32], F32) for i in range(2)]
            m8 = [X.a("m8%d" % i, [128, 8], F32) for i in range(2)]
            negst = [X.a("negst%d" % i, [128, 96], BF16) for i in range(2)]
            qz = [[X.a("qz%d%d" % (a_, b_), [128, 512], BF16) for b_ in range(2)] for a_ in range(2)]
            ftmp1 = X.a("ftmp", [128, 4, 64], F32)
            ftmp = [ftmp1, ftmp1]
            itmp1 = X.a("itmp", [128, 4, 32], F32)
            itmp = [itmp1, itmp1]
            for a_ in range(2):
                for b_ in range(2):
                    S.op('pool', lambda e, a_=a_, b_=b_: e.memset(qz[a_][b_][:], 0.0), writes=[('qz', a_, b_)])
            for i in range(2):
                S.op('pool', lambda e, i=i: e.memset(negst[i][:], 0.0), writes=[('negst', i)])
            state = {'u': 0, 'ps': 0, 'pe': 0}
            pS4 = pfall[:, 0:2048]
            pO = [pf[4], pf[5]]
            pT = pf[6]
            pbs = pf[7][:, :].bitcast(BF16)

            def attn_branch(heads, kts_fn, lhsT_fn, rhs_fn, v_fn, M, mask_fn, fin_fn, extra_reads_fn=None, prep_fn=None,
                            unit_hook=None, v_key=None, cp_eng=None):
                units = [(h, qc) for h in heads for qc in range(4)]
                groups = []
                for ui_, (h, qc) in enumerate(units):
                    prs = kts_fn(qc)
                    u = state['u']
                    state['u'] += 1
                    for pi_, pr in enumerate(prs):
                        groups.append([h, qc, pr, pi_ == 0, pi_ == len(prs) - 1, u, ui_, None])
                if prep_fn is not None:
                    prep_fn(*units[0])
                LAGP = 2
                DEFER = 2
                pending = []
                n = len(groups)
                for i in range(n + LAGP):
                    if i < n:
                        h, qc, pr, first, last, u, ui_, _ = groups[i]
                        if first:
                            if prep_fn is not None and ui_ + 1 < len(units):
                                prep_fn(*units[ui_ + 1])
                            if unit_hook is not None:
                                unit_hook(ui_)
                        slot = state['ps'] % 2
                        state['ps'] += 1
                        eb = state['pe'] % NEP
                        state['pe'] += 1
                        groups[i][7] = eb
                        tl, mk = pr
                        w = sum(c1 - c0 for _, c0, c1 in tl)
                        xr = extra_reads_fn(h, qc) if extra_reads_fn else []
                        wa = tl[0][2] - tl[0][1]
                        split = len(tl) == 2 and wa < 512 and w > 512
                        wo_ = 0
                        addmode = mk is not None and len(mk) == 3
                        mo_ = 0
                        for kt, c0, c1 in tl:
                            dstp = pS4[:, slot * 1024 + wo_:slot * 1024 + wo_ + (c1 - c0)]
                            MM(dstp, lhsT_fn(h, kt), rhs_fn(h, qc)[:, c0:c1], True, not addmode, xr, [('pS', slot)])
                            if addmode:
                                MM(dstp, identb[:, :], mk[0][:, mo_:mo_ + (c1 - c0)], False, True, [mk[1], 'identb'], [('pS', slot)])
                            mo_ += c1 - c0
                            wo_ = 512 if split else wo_ + (c1 - c0)
                        if split:
                            assert w == 2 * wa
                            ACT(Ep[eb][:, 0:w].rearrange("p (t c) -> p t c", t=2),
                                pS4[:, slot * 1024:(slot + 1) * 1024].rearrange("p (t c) -> p t c", t=2)[:, :, 0:wa],
                                AF.Exp, [('pS', slot)], [('E', eb)], scale=0.125)
                        else:
                            ACT(Ep[eb][:, 0:w], pS4[:, slot * 1024:slot * 1024 + w], AF.Exp, [('pS', slot)], [('E', eb)], scale=0.125)
                        if mk is not None and not addmode:
                            mt_, mkey = mk
                            TT('dve', Ep[eb][:, 0:w], Ep[eb][:, 0:w], mt_, ALU.mult, [('E', eb), mkey], [('E', eb)])
                    j = i - LAGP
                    if j >= 0:
                        h, qc, pr, first, last, u, ui_, eb = groups[j]
                        ob = u % 2
                        tl, mk = pr
                        wo_ = 0
                        for t_, (kt, c0, c1) in enumerate(tl):
                            MM(pO[ob][0:M, c0:c1], v_fn(h, kt), Ep[eb][:, wo_:wo_ + (c1 - c0)],
                               first and t_ == 0, last and t_ == len(tl) - 1, [('E', eb)] + ([v_key] if v_key else []), [('pO', ob)])
                            wo_ += c1 - c0
                        if last:
                            for pi_ in [p_ for p_ in pending if p_[2] == ob]:
                                pending.remove(pi_)
                                pi_[1]()
                            CP(cp_eng or ('act' if u % 3 != 2 else 'dve'), otsb[ob][0:M, :], pO[ob][0:M, :], [('pO', ob)], [('otsb', ob)])

                            def fin_late(h=h, qc=qc, ob=ob):
                                for jj in range(4):
                                    TR(pT[:, jj * M:(jj + 1) * M], otsb[ob][0:M, jj * 128:(jj + 1) * 128], identf[0:M, 0:M],
                                       [('otsb', ob), 'identf'], ['pT'])
                                pT3 = pT[:, 0:4 * M].rearrange("p (j m) -> p j m", j=4)
                                TS('dve', rec4[ob][:, :], pT3[:, :, 64], 1e-30, None, ALU.max, None, ['pT'], [('rec4', ob)])
                                S.op('dve', lambda e, ob=ob: e.reciprocal(out=rec4[ob][:, :], in_=rec4[ob][:, :]),
                                     [('rec4', ob)], [('rec4', ob)])
                                fin_fn(h, qc, pT3, ob)
                            pending.append((i + DEFER, fin_late, ob))
                    while pending and pending[0][0] <= i:
                        pending.pop(0)[1]()
                while pending:
                    pending.pop(0)[1]()

            def bc4(ap4, n):
                return ap4.unsqueeze(2).to_broadcast([128, 4, n])

            def finA(h, qc, pT3, ob):
                TT('dve', big[:, 4 * qc:4 * qc + 4, h * 64:(h + 1) * 64], pT3[:, :, 0:64], bc4(rec4[ob][:, :], 64), ALU.mult,
                   ['pT', ('rec4', ob)], [('big', 4 * qc + jj, h) for jj in range(4)])

            def diag_pairs(qc, m0, m1):
                k0 = 4 * qc
                return [([(k0, 0, 512), (k0 + 1, 128, 512)], m0), ([(k0 + 2, 256, 512), (k0 + 3, 384, 512)], m1)]

            def pairsA(qc):
                prs = []
                for kt in range(0, 4 * qc, 2):
                    d0 = 4 * qc - kt
                    off = MA_OFF[6 if d0 >= 6 else d0]
                    prs.append(([(kt, 0, 512), (kt + 1, 0, 512)], (masks[:, off:off + 1024], 'masksA', 'add')))
                return prs + diag_pairs(qc, (masks[:, MA_OFF[0]:MA_OFF[0] + 896], 'masksA', 'add'), (masks[:, MA_OFF[-2]:MA_OFF[-2] + 384], 'masksA', 'add'))

            def pairs_sel(qc):
                prs = [([(kt, 0, 512), (kt + 1, 0, 512)], None) for kt in range(0, 4 * qc, 2)]
                return prs + diag_pairs(qc, (masksN[:, MN_DIAG[0]:MN_DIAG[0] + 896], 'masksN'), (masksN[:, MN_DIAG[1]:MN_DIAG[1] + 384], 'masksN'))

            def pairs_win(qc):
                if qc == 0:
                    return diag_pairs(qc, (masksN[:, MN_DIAG[0]:MN_DIAG[0] + 896], 'masksN'), (masksN[:, MN_DIAG[1]:MN_DIAG[1] + 384], 'masksN'))
                kd = lambda dl: 4 * qc - dl
                return [([(kd(1), 0, 512), (kd(0), 0, 512)], (masksN[:, MN_WIN[0]:MN_WIN[0] + 1024], 'masksN', 'add')),
                        ([(kd(2), 0, 384), (kd(-1), 128, 512)], (masksN[:, MN_WIN[1]:MN_WIN[1] + 768], 'masksN', 'add')),
                        ([(kd(3), 0, 256), (kd(-2), 256, 512)], (masksN[:, MN_WIN[2]:MN_WIN[2] + 512], 'masksN', 'add')),
                        ([(kd(4), 0, 128), (kd(-3), 384, 512)], (masksN[:, MN_WIN[3]:MN_WIN[3] + 256], 'masksN', 'add'))]

            def pairs_cmp(qc):
                return [([(0, 0, 512)], (maskcmp[:, 512 * qc:512 * (qc + 1)], 'maskcmp'))]

            def fin_nsa(br, is_first=False):
                def fin(h, qc, pT3, ob):
                    g = h // 4
                    r = h % 4
                    TT('dve', w4[ob][:, :], rec4[ob][:, :], gates[:, 4 * qc:4 * qc + 4, h * 3 + br], ALU.mult,
                       [('rec4', ob)], [('w4', ob)])
                    dst = big[:, 4 * qc:4 * qc + 4, 512 + h * 64:512 + (h + 1) * 64]
                    dk = [('big', 4 * qc + jj, 8 + h) for jj in range(4)]
                    if is_first:
                        TT('dve', dst, pT3[:, :, 0:64], bc4(w4[ob][:, :], 64), ALU.mult, ['pT', ('w4', ob)], dk)
                    else:
                        TT('dve', ftmp[ob][:, :, :], pT3[:, :, 0:64], bc4(w4[ob][:, :], 64), ALU.mult, ['pT', ('w4', ob)], ['ftmp'])
                        TT('pool', dst, dst, ftmp[ob][:, :, :], ALU.add, ['ftmp'] + dk, dk)
                    if br == 0:
                        idst = imp[:, 4 * qc:4 * qc + 4, g, :]
                        ik = [('imp', 4 * qc + jj, g) for jj in range(4)]
                        if r == 0:
                            TT('dve', idst, pT3[:, :, 65:97], bc4(rec4[ob][:, :], 32), ALU.mult, ['pT', ('rec4', ob)], ik)
                        else:
                            TT('dve', itmp[ob][:, :, :], pT3[:, :, 65:97], bc4(rec4[ob][:, :], 32), ALU.mult, ['pT', ('rec4', ob)], ['itmp'])
                            TT('pool', idst, idst, itmp[ob][:, :, :], ALU.add, ['itmp'] + ik, ik)
                return fin

            heads = list(range(8))
            attn_branch(heads, pairs_cmp,
                        lambda h, kt: kcmpT[:, h // 4, :],
                        lambda h, qc: qbT[:, h, qc * 512:(qc + 1) * 512],
                        lambda h, kt: VC[:, h // 4, :], 97, None, fin_nsa(0, True),
                        extra_reads_fn=lambda h, qc: [('neg', h // 4, 4 * qc + jj) for jj in range(4)], cp_eng='act')
            def selection_a(qt, g):
                sb_ = (qt * 2 + g) % 2
                TT('dve', sc[sb_][:, :], imp[:, qt, g, :], addtab[:, qt, :], ALU.add, [('imp', qt, g), 'addtab'], [('sc', sb_)])
                S.op('dve', lambda e, sb_=sb_: e.max(out=m8[sb_][:, :], in_=sc[sb_][:, :]), [('sc', sb_)], [('m8', sb_)])
                TS('dve', sc[sb_][:, :], sc[sb_][:, :], m8[sb_][:, 7:8], None, ALU.is_ge, None, [('sc', sb_), ('m8', sb_)], [('sc', sb_)])
                TS('dve', negst[sb_][:, 64:96], sc[sb_][:, :], -1.0, -NEGBIG, ALU.add, ALU.mult, [('sc', sb_)], [('negst', sb_)])

            def selection_b(qt, g):
                sb_ = (qt * 2 + g) % 2
                TR(pbs[0:96, 0:128], negst[sb_][:, 0:96], identb[:], [('negst', sb_), 'identb'], ['pbs'])
                for r in range(4):
                    CP('act', qbT[64:96, g * 4 + r, qt * 128:(qt + 1) * 128], pbs[64:96, 0:128],
                       ['pbs', 'qbneg'], [('neg', g, qt)])

            def win_hook(ui_):
                if ui_ >= 1:
                    selection_b((ui_ - 1) % NT, (ui_ - 1) // NT)
                selection_a(ui_ % NT, ui_ // NT)

            qzs = {'n': [0, 0], 'cur': {}}

            def prepA(h, qc):
                par = h % 2
                b_ = qzs['n'][par] % 2
                qzs['n'][par] += 1
                qzs['cur'][(h, qc)] = (par, b_)
                rows = slice(par * 64, par * 64 + 64)
                CP('pool', qz[par][b_][rows, :], qaT[rows, h // 2, qc * 512:(qc + 1) * 512], ['qaT'], [('qz', par, b_)])

            attn_branch(heads, pairsA,
                        lambda h, kt: kaT[:, h // 2, kt * 128:(kt + 1) * 128],
                        lambda h, qc: qz[qzs['cur'][(h, qc)][0]][qzs['cur'][(h, qc)][1]][:, :],
                        lambda h, kt: vA[:, kt, h, :], 65, None, finA,
                        extra_reads_fn=lambda h, qc: [('qz',) + qzs['cur'][(h, qc)]], prep_fn=prepA, v_key='vA_r', unit_hook=win_hook)
            Wo = nc.alloc_sbuf_tensor_at("Wo", [128, 8, D], BF16, offset=BASE + 2 * 4 * T * 2)
            if stage >= 4:
                wo_v = dr['w_out'].rearrange("(kc p) n -> p kc n", p=128)
                for kc in range(0, 8, 4):
                    S.dma('pool', Wo[:, kc:kc + 4, :], wo_v[:, kc:kc + 4, :], writes=['vA_r'])
            selection_b(NT - 1, 1)
            S.dma('pool', masksN[:, :], dr['masksN'], writes=['masksN', 'qaT'])
            attn_branch(heads, pairs_win,
                        lambda h, kt: kwinT[:, h // 4, kt * 128:(kt + 1) * 128],
                        lambda h, qc: qbT[:, h, qc * 512:(qc + 1) * 512],
                        lambda h, kt: vwin[:, kt, h // 4, :], 65, None, fin_nsa(2),
                        extra_reads_fn=lambda h, qc: [('neg', h // 4, 4 * qc + jj) for jj in range(4)], cp_eng='dve')
            attn_branch(heads, pairs_sel,
                        lambda h, kt: kselT[:, h // 4, kt * 128:(kt + 1) * 128],
                        lambda h, qc: qbT[:, h, qc * 512:(qc + 1) * 512],
                        lambda h, kt: vsel[:, kt, h // 4, :], 65, None, fin_nsa(1),
                        extra_reads_fn=lambda h, qc: [('neg', h // 4, 4 * qc + jj) for jj in range(4)], cp_eng='dve')
            S.barrier()
            if stage == 3:
                dump = [('big', big[:, :, :]), ('imp', imp[:, :, :, :]), ('qbT', qbT[0:96, :, :])]

        if stage >= 4:
            P = Arena(BASE, QEND)
            mixedT = P.a("mixedT", [128, 8, T], BF16)
            assert P.p == BASE + 2 * 4 * T * 2
            P.p += 8 * D * 2
            gab = P.a("gab", [128, D], F32)
            gmlp = P.a("gmlp", [128, D], F32)
            gfin = P.a("gfin", [128, D], F32)
            hnT = P.a("hnT", [128, 8, T], BF16)
            mixb = [P.a("mixb%d" % i, [128, D], BF16) for i in range(3)]
            mixb2 = [P.a("mixc%d" % i, [128, D], BF16) for i in range(3)]
            X2 = Arena(XB + NT * D * 4, XL)
            Wu = [X2.a("Wu0", [128, 8, 512], BF16), nc.alloc_sbuf_tensor_at("Wu1", [128, 8, 512], BF16, offset=BASE)]
            Wd = [X2.a("Wd0", [128, 4, D], BF16), nc.alloc_sbuf_tensor_at("Wd1", [128, 4, D], BF16, offset=BASE + 8192)]
            actT = [X2.a("actT%d" % i, [128, 4, 512], BF16) for i in range(2)]
            rl = [X2.a("rl%d" % i, [128, 512], BF16) for i in range(2)]
            junk2 = X2.a("junk2", [128, D], BF16)
            xres = [X2.a("xres0", [128, D], F32)]
            ssA = stat[:, 48:64]
            ssB = stat[:, 64:80]
            tA = stat[:, 80:112].rearrange("p (t k) -> p t k", k=2)
            rA = stat[:, 112:144].rearrange("p (t k) -> p t k", k=2)
            ss2 = stat[:, 144:160]
            t2 = stat[:, 160:176]
            r2 = stat[:, 176:192]
            ss3 = stat[:, 192:208]
            t3 = stat[:, 208:224]
            r3 = stat[:, 224:240]
            S.dma('sp', gab[:, 0:512], dr['g_out_a'].partition_broadcast(128), writes=['gab0'])
            S.dma('sp', gab[:, 512:1024], dr['g_out_b'].partition_broadcast(128), writes=['gab1'])
            S.dma('sp', gmlp[:], dr['norm_mlp'].partition_broadcast(128), writes=['gmlp'])
            S.dma('sp', gfin[:], dr['norm_final'].partition_broadcast(128), writes=['gfin'])
            wu_v = dr['w_up'].rearrange("(kc p) n -> p kc n", p=128)
            wd_v = dr['w_down'].rearrange("(fc p) n -> p fc n", p=128)
            allmt = [('mixedT', t_) for t_ in range(NT)]

            def load_mlp(fg):
                b = fg % 2
                extra = allmt if b == 1 else []
                S.dma('pool', Wu[b][:, :, :], wu_v[:, :, fg * 512:(fg + 1) * 512], writes=[('Wu', b)] + extra)
                S.dma('pool', Wd[b][:, :, :], wd_v[:, fg * 4:(fg + 1) * 4, :], writes=[('Wd', b)] + extra)
            load_mlp(0)

            def rstd_of(ssap, tap, rap, n, rd, wr):
                ACT(tap, ssap, AF.Sqrt, rd + ['epst'], wr, scale=1.0 / n, bias=epst[:, 0:1])
                S.op('dve', lambda e: e.reciprocal(out=rap, in_=tap), wr, wr)

            pbx = [pf[3][:, :].bitcast(BF16), pf[4][:, :].bitcast(BF16)]

            def st_A1(tt):
                bigr = [('big', tt, i) for i in range(16)]
                ACT(junk2[:, 0:512], big[:, tt, 0:512], AF.Square, bigr, ['junk2', ('ssA', tt)], accum_out=ssA[:, tt:tt + 1])
                ACT(junk2[:, 512:1024], big[:, tt, 512:1024], AF.Square, bigr, ['junk2', ('ssB', tt)], accum_out=ssB[:, tt:tt + 1])
                ACT(tA[:, tt, 0:1], ssA[:, tt:tt + 1], AF.Sqrt, [('ssA', tt), 'epst'], [('rA', tt, 0)], scale=1.0 / 512, bias=epst[:, 0:1])
                ACT(tA[:, tt, 1:2], ssB[:, tt:tt + 1], AF.Sqrt, [('ssB', tt), 'epst'], [('rA', tt, 1)], scale=1.0 / 512, bias=epst[:, 0:1])

            def st_A2(tt):
                b = tt % 3
                bigr = [('big', tt, i) for i in range(16)]
                S.op('dve', lambda e: e.reciprocal(out=rA[:, tt, :], in_=tA[:, tt, :]), [('rA', tt, 0), ('rA', tt, 1)], [('rA', tt, 0), ('rA', tt, 1)])
                STT(mixb[b][:, 0:512], big[:, tt, 0:512], rA[:, tt, 0:1], gab[:, 0:512], ALU.mult, ALU.mult,
                    bigr + [('rA', tt, 0), 'gab0'], [('mixb', b, 0)])
                STT(mixb[b][:, 512:1024], big[:, tt, 512:1024], rA[:, tt, 1:2], gab[:, 512:1024], ALU.mult, ALU.mult,
                    bigr + [('rA', tt, 1), 'gab1'], [('mixb', b, 1)])
                S.dma('sp', big[:, tt, :], dr['x'][tt * 128:(tt + 1) * 128, :], writes=bigr)

            def st_B1(tt):
                b = tt % 3
                pbb = tt % 2
                for kc in range(8):
                    TR(pbx[pbb][:, kc * 128:(kc + 1) * 128], mixb[b][:, kc * 128:(kc + 1) * 128], identb[:],
                       [('mixb', b, 0), ('mixb', b, 1), 'identb'], [('pf', 3 + pbb)])

            def st_B2(tt):
                pbb = tt % 2
                CP('act', mixedT[:, :, tt * 128:(tt + 1) * 128], pbx[pbb][:, :].rearrange("p (k t) -> p k t", k=8),
                   [('pf', 3 + pbb)], [('mixedT', tt)])

            opi = [0]

            obanks = [0, 1, 2, 5]
            cbank = {}

            def st_Cmm(tt):
                tsl = slice(tt * 128, (tt + 1) * 128)
                for cg in range(2):
                    bi = obanks[opi[0] % 4]
                    opi[0] += 1
                    cbank[(tt, cg)] = bi
                    for kc in range(8):
                        MM(pf[bi][:, :], mixedT[:, kc, tsl], Wo[:, kc, cg * 512:(cg + 1) * 512], kc == 0, kc == 7,
                           [('mixedT', tt), ('Wo', kc)], [('pf', bi)])

            def st_C1(tt):
                bigr = [('big', tt, i) for i in range(16)]
                for cg in range(2):
                    bi = cbank[(tt, cg)]
                    TT('dve', big[:, tt, cg * 512:(cg + 1) * 512], pf[bi][:, :], big[:, tt, cg * 512:(cg + 1) * 512], ALU.add,
                       [('pf', bi)] + bigr, [('hres', tt, cg)] + bigr)

            def st_C2(tt):
                hr = [('hres', tt, 0), ('hres', tt, 1)] + [('big', tt, i) for i in range(16)]
                ACT(junk2[:, :], big[:, tt, :], AF.Square, hr, ['junk2', ('ss2', tt)], accum_out=ss2[:, tt:tt + 1])
                ACT(t2[:, tt:tt + 1], ss2[:, tt:tt + 1], AF.Sqrt, [('ss2', tt), 'epst'], [('r2', tt)], scale=1.0 / D, bias=epst[:, 0:1])

            def st_C3(tt):
                b = tt % 3
                hr = [('hres', tt, 0), ('hres', tt, 1)] + [('big', tt, i) for i in range(16)]
                S.op('dve', lambda e: e.reciprocal(out=r2[:, tt:tt + 1], in_=t2[:, tt:tt + 1]), [('r2', tt)], [('r2', tt)])
                STT(mixb2[b][:, :], big[:, tt, :], r2[:, tt:tt + 1], gmlp[:, :], ALU.mult, ALU.mult,
                    hr + [('r2', tt), 'gmlp'], [('mixc', b)])

            def st_E1(tt):
                b = tt % 3
                pbb = tt % 2
                for kc in range(8):
                    TR(pb[pbb][:, kc * 128:(kc + 1) * 128], mixb2[b][:, kc * 128:(kc + 1) * 128], identb[:],
                       [('mixc', b), 'identb'], [('pb', pbb)])

            def st_E2(tt):
                pbb = tt % 2
                CP('act', hnT[:, :, tt * 128:(tt + 1) * 128], pb[pbb][:, :].rearrange("p (k t) -> p k t", k=8),
                   [('pb', pbb)], [('hnT', tt)])

            ui = 0
            di = 0

            def mlp_up(st):
                nonlocal ui
                fg, tc = st // 4, st % 4
                wb_ = fg % 2
                ab = st % 2
                hn_r = [('hnT', 4 * tc + jj) for jj in range(4)]
                for ft in range(4):
                    bank = pf[ui % 3]
                    bk = ('pf', ui % 3)
                    rb = ui % 2
                    ui += 1
                    for kc in range(8):
                        MM(bank[:, :], Wu[wb_][:, kc, ft * 128:(ft + 1) * 128], hnT[:, kc, tc * 512:(tc + 1) * 512],
                           kc == 0, kc == 7, hn_r + [('Wu', wb_)], [bk])
                    ACT(rl[rb][:, :], bank[:, :], AF.Relu, [bk], [('rl', rb)])
                    TT('pool', actT[ab][:, ft, :], rl[rb][:, :], rl[rb][:, :], ALU.mult, [('rl', rb)], [('actT', ab, ft)])

            def final_norm(tt):
                hr = [('hres', tt, 0), ('hres', tt, 1)]
                ACT(junk2[:, :], big[:, tt, :], AF.Square, hr, ['junk2', ('ss3', tt)], accum_out=ss3[:, tt:tt + 1])
                rstd_of(ss3[:, tt:tt + 1], t3[:, tt:tt + 1], r3[:, tt:tt + 1], D, [('ss3', tt)], [('r3', tt)])
                STT(big[:, tt, :], big[:, tt, :], r3[:, tt:tt + 1], gfin[:, :], ALU.mult, ALU.mult,
                    hr + [('r3', tt), 'gfin'], hr)
                S.dma('sp', y[tt * 128:(tt + 1) * 128, :], big[:, tt, :], reads=hr)

            def mlp_down(st):
                nonlocal di
                fg, tc = st // 4, st % 4
                wb_ = fg % 2
                ab = st % 2
                for jj in range(4):
                    tt = 4 * tc + jj
                    for cg in range(2):
                        bank = pf[3 + di % 3]
                        bk = ('pf', 3 + di % 3)
                        di += 1
                        for fc in range(4):
                            MM(bank[:, :], actT[ab][:, fc, jj * 128:(jj + 1) * 128], Wd[wb_][:, fc, cg * 512:(cg + 1) * 512],
                               fc == 0, fc == 3, [('actT', ab, fc), ('Wd', wb_)], [bk])
                        TT('dve', big[:, tt, cg * 512:(cg + 1) * 512], bank[:, :], big[:, tt, cg * 512:(cg + 1) * 512], ALU.add,
                           [bk, ('hres', tt, cg)], [('hres', tt, cg)])
                    if fg == 7:
                        final_norm(tt)

            def mlp_step(st):
                if st < 32:
                    mlp_up(st)
                if st >= 1:
                    mlp_down(st - 1)
                    if (st - 1) % 4 == 3 and (st - 1) // 4 + 2 < 8:
                        load_mlp((st - 1) // 4 + 2)

            nst = 0
            stages = [st_A1, st_A2, st_B1, st_B2, st_Cmm, st_C1, st_C2, st_C3, st_E1, st_E2]
            for it in range(NT + len(stages) - 1):
                for si, stg_fn in enumerate(stages):
                    tt = it - si
                    if 0 <= tt < NT:
                        stg_fn(tt)
                if it in (21, 22, 23):
                    mlp_step(nst)
                    nst += 1
            load_mlp(1)
            for st in range(nst, 33):
                mlp_step(st)

        for name, ap in dump:
            if name in tap_dr:
                S.dma('pool', tap_dr[name], ap)
        S.emit()
    nc._sched_log = S.log
    return nc


_CACHE = {}


def kernel(**inputs):
    consts = _host_consts()
    if 'nc' not in _CACHE:
        _CACHE['nc'] = build_nc()
    nc = _CACHE['nc']
    x = np.ascontiguousarray(np.asarray(inputs['x'], dtype=np.float32))
    shared = {}
    for k, shp in W_SHAPES.items():
        shared[k] = np.ascontiguousarray(np.asarray(inputs[k], dtype=np.float32).reshape(shp))
    for k in CONST_SHAPES:
        shared[k] = consts[k]
    in_maps = []
    for c in range(8):
        m = dict(shared)
        m['x'] = x[c]
        in_maps.append(m)
    res = run_bass_kernel_spmd(nc, in_maps, core_ids=list(range(8)))
    out = np.stack([np.asarray(res.results[c]['y'], dtype=np.float32) for c in range(8)], axis=0)
    return out
```
